# Optimizing a Trainium2 kernel written in Bass

```python
import math
import jax, jax.numpy as jnp
from jax import lax
import numpy as np

D_MODEL = 2048
BATCH = 4
SEQ = 4096
DEPTH = 1

HEAD_DIM = 128
SB_HEADS = D_MODEL // (2 * HEAD_DIM)
RET_HEADS = D_MODEL // (2 * HEAD_DIM)
SB_WIDTH = SB_HEADS * HEAD_DIM
RET_WIDTH = RET_HEADS * HEAD_DIM
MIX_WIDTH = SB_WIDTH + RET_WIDTH
IN_COLS = 3 * SB_WIDTH + 4 * RET_WIDTH
Q_BLOCK = 128
RET_CHUNK = 128
ROPE_BASE = 10000.0
N_EXPERTS = 64
TOP_K = 8
N_GROUPS = 8
TOPK_GROUP = 4
EXPERT_DIM = D_MODEL // 4
SHARED_DIM = D_MODEL // 4
ROUTED_SCALE = 2.5
MOE_BLOCK = 128
EPS = 1e-6

kernel_name = 'hybrid_sbattn_retention_moe_adaln'


def rmsnorm(x, g):
    xf = x.astype(jnp.float32)
    y = xf * lax.rsqrt(jnp.mean(xf * xf, axis=-1, keepdims=True) + EPS)
    return (y * g.astype(jnp.float32)).astype(x.dtype)


def modulate(h, shift, scale):
    return h * (1.0 + scale[:, None, :]) + shift[:, None, :]


def rope_tables(S):
    pos = jnp.arange(S, dtype=jnp.float32)
    inv_freq = ROPE_BASE ** (-jnp.arange(0, HEAD_DIM, 2, dtype=jnp.float32) / HEAD_DIM)
    ang = pos[:, None] * inv_freq[None, :]
    return jnp.cos(ang)[:, None, :], jnp.sin(ang)[:, None, :]


def apply_rope(t, cos, sin):
    t1, t2 = t[..., :HEAD_DIM // 2], t[..., HEAD_DIM // 2:]
    return jnp.concatenate([t1 * cos - t2 * sin, t1 * sin + t2 * cos], axis=-1)


def stick_breaking_attention(q, k, v):
    B, S, H, dh = q.shape
    qf = jnp.transpose(q.astype(jnp.float32), (0, 2, 1, 3)) * (dh ** -0.5)
    kf = jnp.transpose(k.astype(jnp.float32), (0, 2, 1, 3))
    vf = jnp.transpose(v.astype(jnp.float32), (0, 2, 1, 3))
    nq = S // Q_BLOCK
    qb = jnp.transpose(qf.reshape(B, H, nq, Q_BLOCK, dh), (2, 0, 1, 3, 4))
    key_pos = jnp.arange(S, dtype=jnp.int32)

    def block(args):
        q_blk, t0 = args
        z = jnp.einsum('bhqd,bhkd->bhqk', q_blk, kf)
        qpos = t0 + jnp.arange(Q_BLOCK, dtype=jnp.int32)
        causal = key_pos[None, :] < qpos[:, None]
        log_surv = jnp.where(causal, jax.nn.log_sigmoid(-z), 0.0)
        after = lax.cumsum(log_surv, axis=3, reverse=True) - log_surv
        a = jnp.where(causal, jnp.exp(jax.nn.log_sigmoid(z) + after), 0.0)
        return jnp.einsum('bhqk,bhkd->bhqd', a, vf)

    ob = lax.map(block, (qb, jnp.arange(nq, dtype=jnp.int32) * Q_BLOCK))
    return jnp.transpose(ob, (1, 0, 3, 2, 4)).reshape(B, S, H * dh)


def chunkwise_retention(q, k, v):
    B, S, H, dh = q.shape
    C = RET_CHUNK
    N = S // C

    def to_chunks(t):
        return jnp.transpose(t.astype(jnp.float32), (0, 2, 1, 3)).reshape(B, H, N, C, dh)

    qc, kc, vc = to_chunks(q), to_chunks(k) * (dh ** -0.5), to_chunks(v)
    log_g = jnp.log(1.0 - jnp.exp2(-5.0 - jnp.arange(H, dtype=jnp.float32)))
    i = jnp.arange(C, dtype=jnp.float32)
    diff = i[:, None] - i[None, :]
    lower = diff >= 0
    dmat = jnp.where(lower[None], jnp.exp(jnp.where(lower, diff, 0.0)[None] * log_g[:, None, None]), 0.0)
    scores = jnp.einsum('bhnid,bhnjd->bhnij', qc, kc) * dmat[None, :, None]
    intra = jnp.einsum('bhnij,bhnje->bhnie', scores, vc)
    k_decay = jnp.exp((C - 1.0 - i)[None, :] * log_g[:, None])
    chunk_kv = jnp.einsum('bhncd,bhnce->bhnde', kc * k_decay[None, :, None, :, None], vc)
    chunk_decay = jnp.exp(C * log_g)[None, :, None, None]

    def step(state, kv):
        return state * chunk_decay + kv, state

    _, r_prev = lax.scan(step, jnp.zeros((B, H, dh, dh), jnp.float32), jnp.moveaxis(chunk_kv, 2, 0))
    r_prev = jnp.moveaxis(r_prev, 0, 2)
    q_decay = jnp.exp((i + 1.0)[None, :] * log_g[:, None])
    cross = jnp.einsum('bhncd,bhnde->bhnce', qc * q_decay[None, :, None, :, None], r_prev)
    out = (intra + cross).reshape(B, H, S, dh)
    return jnp.transpose(out, (0, 2, 1, 3))


def token_mixer(h, w_in, ret_gn_g, w_out, cos, sin):
    B, S, _ = h.shape
    u = h @ w_in
    cuts = [SB_WIDTH, 2 * SB_WIDTH, 3 * SB_WIDTH,
            3 * SB_WIDTH + RET_WIDTH, 3 * SB_WIDTH + 2 * RET_WIDTH, 3 * SB_WIDTH + 3 * RET_WIDTH]
    sbq, sbk, sbv, rq, rk, rv, rg = jnp.split(u, cuts, axis=-1)
    heads = lambda t, n: t.reshape(B, S, n, HEAD_DIM)
    sb = stick_breaking_attention(heads(sbq, SB_HEADS), heads(sbk, SB_HEADS), heads(sbv, SB_HEADS))
    ret = chunkwise_retention(apply_rope(heads(rq, RET_HEADS).astype(jnp.float32), cos, sin),
                              apply_rope(heads(rk, RET_HEADS).astype(jnp.float32), cos, sin),
                              heads(rv, RET_HEADS))
    ret = ret * lax.rsqrt(jnp.mean(ret * ret, axis=-1, keepdims=True) + EPS)
    ret = ret.reshape(B, S, RET_WIDTH) * ret_gn_g.astype(jnp.float32) * jax.nn.silu(rg.astype(jnp.float32))
    o = jnp.concatenate([sb.astype(h.dtype), ret.astype(h.dtype)], axis=-1)
    return o @ w_out


def moe_ffn(h2, w_router, router_bias, w_gate, w_up, w_down, ws_gate, ws_up, ws_down):
    T, D = h2.shape
    E = N_EXPERTS
    scores = jax.nn.sigmoid((h2 @ w_router).astype(jnp.float32))
    choice = scores + router_bias.astype(jnp.float32)
    grp = choice.reshape(T, N_GROUPS, E // N_GROUPS)
    grp_score = jnp.sum(lax.top_k(grp, 2)[0], axis=-1)
    _, gidx = lax.top_k(grp_score, TOPK_GROUP)
    gmask = jnp.any(gidx[..., None] == jnp.arange(N_GROUPS)[None, None, :], axis=1)
    emask = jnp.repeat(gmask, E // N_GROUPS, axis=1)
    _, eidx = lax.top_k(jnp.where(emask, choice, -jnp.inf), TOP_K)
    gsel = jnp.take_along_axis(scores, eidx, axis=1)
    gsel = gsel / jnp.sum(gsel, axis=-1, keepdims=True) * ROUTED_SCALE

    A = T * TOP_K
    flat_e = eidx.reshape(-1).astype(jnp.int32)
    flat_t = jnp.repeat(jnp.arange(T, dtype=jnp.int32), TOP_K)
    flat_w = gsel.reshape(-1)
    order = jnp.argsort(flat_e)
    se, st, sw = flat_e[order], flat_t[order], flat_w[order]
    counts = jax.ops.segment_sum(jnp.ones_like(flat_e), flat_e, num_segments=E)
    padded = ((counts + MOE_BLOCK - 1) // MOE_BLOCK) * MOE_BLOCK
    start = jnp.cumsum(counts) - counts
    pend = jnp.cumsum(padded)
    pstart = pend - padded
    pos = pstart[se] + (jnp.arange(A, dtype=jnp.int32) - start[se])
    P = A + E * MOE_BLOCK
    n_blk = P // MOE_BLOCK
    row_tok = jnp.full((P,), T, jnp.int32).at[pos].set(st)
    row_w = jnp.zeros((P,), jnp.float32).at[pos].set(sw)
    blk_expert = jnp.clip(jnp.searchsorted(pend, jnp.arange(n_blk, dtype=jnp.int32) * MOE_BLOCK, side='right'), 0, E - 1)
    h_pad = jnp.concatenate([h2, jnp.zeros((1, D), h2.dtype)], axis=0)

    def expert_block(y, xs):
        b, e = xs
        rows = lax.dynamic_slice(row_tok, (b * MOE_BLOCK,), (MOE_BLOCK,))
        gw = lax.dynamic_slice(row_w, (b * MOE_BLOCK,), (MOE_BLOCK,))
        xb = h_pad[rows]
        out = (jax.nn.silu(xb @ w_gate[e]) * (xb @ w_up[e])) @ w_down[e]
        return y.at[rows].add((out * gw[:, None]).astype(y.dtype)), None

    y, _ = lax.scan(expert_block, jnp.zeros((T + 1, D), h2.dtype),
                    (jnp.arange(n_blk, dtype=jnp.int32), blk_expert))
    shared = (jax.nn.silu(h2 @ ws_gate) * (h2 @ ws_up)) @ ws_down
    return y[:T] + shared


def setup_inputs(seed: int = 0) -> dict:
    key = jax.random.key(seed)
    ks = jax.random.split(key, 24)
    D, E, F, Fs = D_MODEL, N_EXPERTS, EXPERT_DIM, SHARED_DIM

    def nrm(k, shape, fan_in, scale=1.0):
        return jax.random.normal(k, shape, jnp.float32) * (scale * fan_in ** -0.5)

    def gain(k, shape):
        return 1.0 + 0.02 * jax.random.normal(k, shape, jnp.float32)

    return {
        'x': jax.random.normal(ks[0], (BATCH, SEQ, D), jnp.float32),
        'c': jax.random.normal(ks[1], (BATCH, D), jnp.float32),
        'w_ada': nrm(ks[2], (DEPTH, D, 6 * D), D, 0.5),
        'b_ada': 0.02 * jax.random.normal(ks[3], (DEPTH, 6 * D), jnp.float32),
        'norm1_g': gain(ks[4], (DEPTH, D)),
        'w_in': nrm(ks[5], (DEPTH, D, IN_COLS), D),
        'ret_gn_g': gain(ks[6], (DEPTH, RET_WIDTH)),
        'w_out': nrm(ks[7], (DEPTH, MIX_WIDTH, D), MIX_WIDTH),
        'norm2_g': gain(ks[8], (DEPTH, D)),
        'w_router': nrm(ks[9], (DEPTH, D, E), D),
        'router_bias': 0.01 * jax.random.normal(ks[10], (DEPTH, E), jnp.float32),
        'w_gate': nrm(ks[11], (DEPTH, E, D, F), D),
        'w_up': nrm(ks[12], (DEPTH, E, D, F), D),
        'w_down': nrm(ks[13], (DEPTH, E, F, D), F),
        'ws_gate': nrm(ks[14], (DEPTH, D, Fs), D),
        'ws_up': nrm(ks[15], (DEPTH, D, Fs), D),
        'ws_down': nrm(ks[16], (DEPTH, Fs, D), Fs),
        'w_ada_final': nrm(ks[17], (D, 2 * D), D, 0.5),
        'b_ada_final': 0.02 * jax.random.normal(ks[18], (2 * D,), jnp.float32),
        'norm_f_g': gain(ks[19], (D,)),
    }


def reference(x, c, w_ada, b_ada, norm1_g, w_in, ret_gn_g, w_out, norm2_g, w_router, router_bias,
              w_gate, w_up, w_down, ws_gate, ws_up, ws_down, w_ada_final, b_ada_final, norm_f_g):
    B, S, D = x.shape
    cos, sin = rope_tables(S)
    cs = jax.nn.silu(c)
    for l in range(DEPTH):
        mod = cs @ w_ada[l] + b_ada[l]
        sh1, sc1, g1, sh2, sc2, g2 = jnp.split(mod, 6, axis=-1)
        h = modulate(rmsnorm(x, norm1_g[l]), sh1, sc1)
        x = x + g1[:, None, :] * token_mixer(h, w_in[l], ret_gn_g[l], w_out[l], cos, sin)
        h = modulate(rmsnorm(x, norm2_g[l]), sh2, sc2)
        y = moe_ffn(h.reshape(B * S, D), w_router[l], router_bias[l], w_gate[l], w_up[l], w_down[l],
                    ws_gate[l], ws_up[l], ws_down[l])
        x = x + g2[:, None, :] * y.reshape(B, S, D)
    modf = cs @ w_ada_final + b_ada_final
    shf, scf = jnp.split(modf, 2, axis=-1)
    return modulate(rmsnorm(x, norm_f_g), shf, scf)
```

```python
import contextlib
import numpy as np
import concourse.bass as bass
import concourse.mybir as mybir
from concourse.bass_utils import run_bass_kernel_spmd

F32 = mybir.dt.float32
F32R = mybir.dt.float32r
AF = mybir.ActivationFunctionType
ALU = mybir.AluOpType

EPOCH = 30000
D = 2048
KC = 16
SO = 2048
SC = 4096
NE = 64
FF = 512
EPS = 1e-6
ARENA_R = 36864
ARENA_F = 14976


class Buf:
    __slots__ = ("name", "w", "r")

    def __init__(self, name=""):
        self.name = name
        self.w = None
        self.r = []


class DSem:
    def __init__(self, key):
        self.key = key
        self.count = 0


class KB:
    ENGS = ("pe", "act", "dve", "pool", "sp")

    def __init__(self):
        self.nc = bass.Bass("TRN2", target_bir_lowering=False)
        self.nc.dge_precook = False
        self.stack = contextlib.ExitStack()
        self.streams = {e: [] for e in self.ENGS}
        self.seq = {e: 0 for e in self.ENGS}
        self.waited = {e: {} for e in self.ENGS}
        self.semkeys = []
        self.dsems = []
        self.nbuf = 0
        self.rr = 0
        self.free_dsems = []
        self.pool = list(range(8))

    def sb(self, name, shape, dtype=F32):
        return self.stack.enter_context(self.nc.sbuf_tensor(name, list(shape), dtype))

    def ps(self, name, shape, dtype=F32):
        return self.stack.enter_context(self.nc.psum_tensor(name, list(shape), dtype))

    def dsem(self):
        if self.free_dsems:
            return self.free_dsems.pop()
        k = "d%d" % (len(self.dsems) + 1)
        self.semkeys.append(k)
        d = DSem(k)
        self.dsems.append(d)
        return d

    def buf(self, name=""):
        self.nbuf += 1
        return Buf(name or "b%d" % self.nbuf)

    def _need(self, eng, tok, waits):
        if tok is None:
            return
        key, val = tok
        if self.waited[eng].get(key, 0) >= val:
            return
        if eng == "pe" and key.startswith("pe@"):
            return
        waits[key] = max(waits.get(key, 0), val)

    def _deps(self, eng, reads, writes, own_dsem=None):
        waits = {}
        for b in reads:
            self._need(eng, b.w, waits)
        for b in writes:
            if not (own_dsem is not None and b.w is not None and b.w[0] == own_dsem.key):
                self._need(eng, b.w, waits)
            for t in b.r:
                self._need(eng, t, waits)
        for k, v in waits.items():
            self.waited[eng][k] = v
        return sorted(waits.items())

    def _mark(self, tok, reads, writes):
        for b in reads:
            b.r.append(tok)
        for b in writes:
            b.w = tok
            b.r = []

    def _engtok(self, eng):
        s = self.seq[eng]
        if s == 0:
            return None
        ep = (s - 1) // EPOCH
        return ("%s@%d" % (eng, ep), s - ep * EPOCH)

    def op(self, eng, fn, reads=(), writes=()):
        waits = self._deps(eng, reads, writes)
        self.seq[eng] += 1
        tok = self._engtok(eng)
        if tok[0] not in self.semkeys:
            self.semkeys.append(tok[0])
        self.streams[eng].append((waits, fn, tok[0], 1))
        self._mark(tok, reads, writes)
        return tok

    def dma(self, q, out, in_, dsem, reads=(), writes=(), **kw):
        waits = self._deps(q, reads, writes, own_dsem=dsem)
        dsem.count += 16
        tok = (dsem.key, dsem.count)
        self.streams[q].append((waits, lambda e: e.dma_start(out=out, in_=in_, **kw), dsem.key, 16))
        self._mark(tok, reads, writes)
        return tok

    def wait_tok(self, eng, tok):
        waits = {}
        self._need(eng, tok, waits)
        for k, v in waits.items():
            self.waited[eng][k] = v
        if waits:
            self.streams[eng].append((sorted(waits.items()), None, None, 0))

    def barrier(self):
        toks = [self._engtok(e) for e in self.ENGS]
        toks += [(d.key, d.count) for d in self.dsems if d.count > 0]
        for e in self.ENGS:
            for t in toks:
                self.wait_tok(e, t)
        self.free_dsems = list(self.dsems)

    def build(self):
        nc = self.nc
        sems = {}
        for k in self.semkeys:
            sems[k] = self.stack.enter_context(nc.semaphore(k.replace("@", "_")))
        streams = self.streams

        def replay(name):
            def f(e):
                for waits, fn, key, amt in streams[name]:
                    for wk, wv in waits:
                        e.wait_ge(sems[wk], wv)
                    if fn is not None:
                        fn(e).then_inc(sems[key], amt)
            return f

        with nc.Block() as block:
            block.tensor(replay("pe"))
            block.scalar(replay("act"))
            block.vector(replay("dve"))
            block.gpsimd(replay("pool"))
            block.sync(replay("sp"))
        self.stack.close()
        return nc


class Arena:
    def __init__(self, k, tf, tr):
        self.k, self.tf, self.tr = k, tf, tr
        self.offf = self.offr = 0

    def reset(self):
        self.offf = self.offr = 0

    def alloc(self, n, dtype=F32):
        if dtype is F32R:
            assert self.offr + n <= ARENA_R, ("arena R overflow", self.offr, n)
            ap = self.tr[:, self.offr:self.offr + n]
            self.offr += n
        else:
            assert self.offf + n <= ARENA_F, ("arena F overflow", self.offf, n)
            ap = self.tf[:, self.offf:self.offf + n]
            self.offf += n
        return ap, self.k.buf()


def r32(ap):
    return ap.bitcast(F32R)


def f32(ap):
    return ap.bitcast(F32)


def build_program(stage=99, debug=False):
    k = KB()
    nc = k.nc
    kind_s = "ExternalOutput" if debug else "Internal"

    def din(name, shape):
        return nc.dram_tensor(name, list(shape), F32, kind="ExternalInput").ap()

    def dscr(name, shape):
        return nc.dram_tensor(name, list(shape), F32, kind=kind_s).ap()

    xr = din("xr", [SC, D])
    flagc = din("flagc", [128, 1])
    ccol = din("ccol", [128, KC])
    w_ada = din("w_ada", [D, 6 * D])
    b_ada = din("b_ada", [1, 6 * D])
    w_adaf = din("w_adaf", [D, 2 * D])
    b_adaf = din("b_adaf", [1, 2 * D])
    g1col = din("g1col", [128, KC])
    g2col = din("g2col", [128, KC])
    gfrow = din("gfrow", [1, D])
    ggn = din("ggn", [128, 8])
    w_in = din("w_in", [D, 7168])
    w_out = din("w_out", [D, D])
    w_router = din("w_router", [D, NE])
    rbias = din("rbias", [128, NE])
    w_gate = din("w_gate", [NE, D, FF])
    w_up = din("w_up", [NE, D, FF])
    w_down = din("w_down", [NE, FF, D])
    ws_gate = din("ws_gate", [D, FF])
    ws_up = din("ws_up", [D, FF])
    ws_down = din("ws_down", [FF, D])
    cosT = din("cosT", [128, SC])
    sinT = din("sinT", [128, SC])
    prot = din("prot", [128, 128])
    ident_d = din("ident", [128, 128])
    ones_d = din("ones", [128, 128])
    dmatT = din("dmatT", [128, 8 * 128])
    qdec = din("qdec", [128, 8 * 128])
    kdec = din("kdec", [128, 8])
    cdec = din("cdec", [128, 8])
    pdec = din("pdec", [128, 8 * 16])
    out_d = nc.dram_tensor("out", [SO, D], F32, kind="ExternalOutput").ap()

    modb_d = dscr("modb_d", [128, 8 * D])
    hT_d = dscr("hT_d", [KC, 128, SC])
    qT_d = dscr("qT_d", [8, 128, SO])
    kT_d = dscr("kT_d", [8, 128, SC])
    v_d = dscr("v_d", [SC, 1024])
    rqT_d = dscr("rqT_d", [8, 128, SO])
    rkT_d = dscr("rkT_d", [8, 128, SC])
    rv_d = dscr("rv_d", [SC, 1024])
    rgT_d = dscr("rgT_d", [8, 128, SO])
    oT_d = dscr("oT_d", [16, 128, SO])
    x1_d = dscr("x1_d", [SO, D])
    h2T_d = dscr("h2T_d", [KC, 128, SO])
    y_d = dscr("y_d", [SO, D])

    arena_f = k.sb("arena_f", [128, ARENA_F])
    arena_r = k.sb("arena_r", [128, ARENA_R], F32R)
    ar = Arena(k, arena_f, arena_r)
    ident = k.sb("ident_sb", [128, 128])
    ones = k.sb("ones_sb", [128, 128])
    flag = k.sb("flag_sb", [128, 1])
    cols = k.sb("cols_sb", [128, 4 * KC])
    gw = k.sb("gw_sb", [128, 16 * 65])
    b_ident, b_ones, b_flag, b_cols, b_gw = (k.buf() for _ in range(5))
    PS = [k.ps("ps%d" % i, [128, 512]) for i in range(8)]
    PB = [k.buf("psb%d" % i) for i in range(8)]

    def psnext():
        i = k.pool[k.rr % len(k.pool)]
        k.rr += 1
        return PS[i], PB[i]

    def load(dst, src, dsem, wbuf, rbufs=(), q="sp", **kw):
        return k.dma(q, dst, src, dsem, reads=list(rbufs), writes=[wbuf], **kw)

    def store(dst, src, dsem, rbuf, wbufs=(), q="pool", **kw):
        return k.dma(q, dst, src, dsem, reads=[rbuf], writes=list(wbufs), **kw)

    dc = k.dsem()
    load(ident[:], ident_d, dc, b_ident)
    load(ones[:], ones_d, k.dsem(), b_ones)
    load(flag[:], flagc, k.dsem(), b_flag)

    last_store = [None]
    dram_bufs = {}

    def dbuf(name):
        if name not in dram_bufs:
            dram_bufs[name] = k.buf("dram_" + name)
        return dram_bufs[name]

    ar.reset()
    cc, b_cc = ar.alloc(KC)
    cs, b_cs = ar.alloc(KC)
    csB, b_csB = ar.alloc(KC * 128)
    CW = 256
    wt = [ar.alloc(KC * CW) for _ in range(2)]
    wds = [k.dsem() for _ in range(2)]
    bt = [ar.alloc(CW) for _ in range(2)]
    bds = [k.dsem() for _ in range(2)]
    mst = [ar.alloc(CW) for _ in range(2)]
    mds = [k.dsem() for _ in range(2)]
    load(cc, ccol, k.dsem(), b_cc)
    k.op("act", lambda e: e.activation(out=cs, in_=cc, func=AF.Silu), reads=[b_cc], writes=[b_cs])
    for kc in range(KC):
        k.op("dve", lambda e, kc=kc: e.tensor_scalar(out=csB[:, kc * 128:(kc + 1) * 128], in0=ones[:],
                                                     scalar1=cs[:, kc:kc + 1], scalar2=None, op0=ALU.mult),
             reads=[b_ones, b_cs], writes=[b_csB])
    b_modb = dbuf("modb")
    for ch in range(64):
        if ch < 48:
            wsrc, bsrc, c0 = w_ada, b_ada, ch * CW
        else:
            wsrc, bsrc, c0 = w_adaf, b_adaf, (ch - 48) * CW
        (wa, wb), wd = wt[ch % 2], wds[ch % 2]
        (ba, bb), bd = bt[ch % 2], bds[ch % 2]
        (ma, mb), md = mst[ch % 2], mds[ch % 2]
        wa3 = wa.rearrange("p (k c) -> p k c", k=KC)
        load(wa3, wsrc[:, c0:c0 + CW].rearrange("(k p) c -> p k c", p=128), wd, wb)
        load(ba[0:1, :], bsrc[:, c0:c0 + CW], bd, bb)
        pt, pb = psnext()
        for kc in range(KC):
            k.op("pe", lambda e, pt=pt, kc=kc, wa3=wa3: e.matmul(pt[:, 0:CW], lhsT=csB[:, kc * 128:(kc + 1) * 128],
                                                                 rhs=wa3[:, kc, :], start=(kc == 0), stop=False),
                 reads=[b_csB, wb], writes=[pb])
        k.op("pe", lambda e, pt=pt, ba=ba: e.matmul(pt[:, 0:CW], lhsT=ones[0:1, :], rhs=ba[0:1, :], start=False, stop=True),
             reads=[b_ones, bb], writes=[pb])
        k.op("act", lambda e, pt=pt, ma=ma: e.copy(out=ma, in_=pt[:, 0:CW]), reads=[pb], writes=[mb])
        store(modb_d[:, ch * CW:(ch + 1) * CW], ma, md, mb, wbufs=[b_modb])
    tmpc, b_tmpc = ar.alloc(6 * KC)
    gcol, b_gcol = ar.alloc(2 * KC)
    dcol = k.dsem()
    for i, blk in enumerate([0, 1, 3, 4]):
        load(tmpc[:, i * KC:(i + 1) * KC],
             modb_d[0, blk * D:(blk + 1) * D].rearrange("(k p) -> p k", p=128),
             dcol, b_tmpc, rbufs=[b_modb], allow_slow_non_contiguous=True)
    load(gcol[:, 0:KC], g1col, dcol, b_gcol)
    load(gcol[:, KC:2 * KC], g2col, dcol, b_gcol)
    for j in range(2):
        k.op("dve", lambda e, j=j: e.scalar_tensor_tensor(out=cols[:, (2 * j) * KC:(2 * j + 1) * KC],
                                                          in0=tmpc[:, (2 * j + 1) * KC:(2 * j + 2) * KC], scalar=1.0,
                                                          in1=gcol[:, j * KC:(j + 1) * KC], op0=ALU.add, op1=ALU.mult),
             reads=[b_tmpc, b_gcol], writes=[b_cols])
        k.op("dve", lambda e, j=j: e.tensor_copy(out=cols[:, (2 * j + 1) * KC:(2 * j + 2) * KC],
                                                 in_=tmpc[:, (2 * j) * KC:(2 * j + 1) * KC]),
             reads=[b_tmpc], writes=[b_cols])
    k.barrier()
    if stage <= 0:
        return finish(k, nc)

    def pipeline(items, stages):
        n = len(items)
        for s_ in range(n + len(stages) - 1):
            for d in range(len(stages) - 1, -1, -1):
                if 0 <= s_ - d < n:
                    stages[d](items[s_ - d])

    def norm_phase(ntiles, src_rows, src_rbufs, dstT, b_dst, colbase, router=None):
        xts = [ar.alloc(D) for _ in range(2)]
        xds = [k.dsem() for _ in range(2)]
        junk, b_junk = ar.alloc(D)
        xss = [ar.alloc(D) for _ in range(2)]
        stats = [ar.alloc(4) for _ in range(2)]
        hts = [ar.alloc(KC * 128, F32R) for _ in range(2)]
        hds = [k.dsem() for _ in range(2)]
        if router is not None:
            hF, b_hF = ar.alloc(KC * 128)
            hF3 = hF.rearrange("p (k t) -> p k t", k=KC)

        def n1(t):
            (xt, b_xt), (stat, b_stat) = xts[t % 2], stats[t % 2]
            load(xt, src_rows(t), xds[t % 2], b_xt, rbufs=src_rbufs)
            k.op("dve", lambda e: e.scalar_tensor_tensor(out=junk, in0=xt, scalar=1.0, in1=xt, op0=ALU.mult, op1=ALU.mult,
                                                         accum_out=stat[:, 0:1]), reads=[b_xt], writes=[b_junk, b_stat])
            k.op("dve", lambda e: e.tensor_scalar(out=stat[:, 1:2], in0=stat[:, 0:1], scalar1=1.0 / D, scalar2=EPS,
                                                  op0=ALU.mult, op1=ALU.add), reads=[b_stat], writes=[b_stat])
            k.op("act", lambda e: e.activation(out=stat[:, 2:3], in_=stat[:, 1:2], func=AF.Sqrt), reads=[b_stat], writes=[b_stat])
            k.op("dve", lambda e: e.reciprocal(out=stat[:, 3:4], in_=stat[:, 2:3]), reads=[b_stat], writes=[b_stat])

        def n2(t):
            (xt, b_xt), (stat, b_stat), (xs, b_xs) = xts[t % 2], stats[t % 2], xss[t % 2]
            k.op("act", lambda e: e.activation(out=xs, in_=xt, func=AF.Identity, scale=stat[:, 3:4]), reads=[b_xt, b_stat], writes=[b_xs])

        def n3(t):
            (xs, b_xs) = xss[t % 2]
            for g in range(4):
                pt, pb = PS[(t % 2) * 4 + g], PB[(t % 2) * 4 + g]
                for j in range(4):
                    kc = g * 4 + j
                    k.op("pe", lambda e, pt=pt, j=j, kc=kc: e.transpose(pt[:, j * 128:(j + 1) * 128], xs[:, kc * 128:(kc + 1) * 128], ident[:]),
                         reads=[b_xs, b_ident], writes=[pb])

        def n4(t):
            (ht, b_ht) = hts[t % 2]
            ht3 = ht.rearrange("p (k t) -> p k t", k=KC)
            for g in range(4):
                pt, pb = PS[(t % 2) * 4 + g], PB[(t % 2) * 4 + g]
                for j in range(4):
                    kc = g * 4 + j
                    k.op("act", lambda e, pt=pt, j=j, kc=kc: e.activation(out=ht3[:, kc, :], in_=pt[:, j * 128:(j + 1) * 128], func=AF.Identity,
                                                                          scale=cols[:, colbase + kc:colbase + kc + 1],
                                                                          bias=cols[:, colbase + KC + kc:colbase + KC + kc + 1]),
                         reads=[pb, b_cols], writes=[b_ht])
                    if router is not None:
                        k.op("act", lambda e, pt=pt, j=j, kc=kc: e.activation(out=hF3[:, kc, :], in_=pt[:, j * 128:(j + 1) * 128], func=AF.Identity,
                                                                              scale=cols[:, colbase + kc:colbase + kc + 1],
                                                                              bias=cols[:, colbase + KC + kc:colbase + KC + kc + 1]),
                             reads=[pb, b_cols], writes=[b_hF])
            store(dstT[:, :, t * 128:(t + 1) * 128].rearrange("k p t -> p k t"), f32(ht3), hds[t % 2], b_ht, wbufs=[b_dst])

        stages = [n1, n2, n3, n4]
        if router is not None:
            stages.append(lambda t: router(t, hF3, b_hF, (PS[(t % 2) * 4], PB[(t % 2) * 4])))
        pipeline(list(range(ntiles)), stages)

    ar.reset()
    b_hTd = dbuf("hT")
    norm_phase(32, lambda t: xr[t * 128:(t + 1) * 128, :], [], hT_d, b_hTd, 0)
    k.barrier()
    if stage <= 1:
        return finish(k, nc)

    ar.reset()
    wts = [ar.alloc(KC * 256, F32R) for _ in range(2)]
    wds2 = [k.dsem() for _ in range(2)]
    hTs = [ar.alloc(KC * 512, F32R) for _ in range(3)]
    hds2 = [k.dsem() for _ in range(3)]
    cs_t, b_cost = ar.alloc(SC)
    sn_t, b_sint = ar.alloc(SC)
    prot_t, b_prot = ar.alloc(128, F32R)
    load(cs_t, cosT, k.dsem(), b_cost)
    load(sn_t, sinT, k.dsem(), b_sint)
    load(prot_t, r32(prot), k.dsem(), b_prot)
    stg = [ar.alloc(512) for _ in range(4)]
    sds = [k.dsem() for _ in range(4)]
    ut, b_ut = ar.alloc(512, F32R)
    t1, b_t1 = ar.alloc(512)
    t2, b_t2 = ar.alloc(512)
    nst = [0]
    nht = [0]
    nw = [0]
    kinds = ["q", "q", "k", "k", "v", "v", "rq", "rq", "rk", "rk", "rv", "rv", "rg", "rg"]
    for (ta_, tb__) in [(0, 1), (2, 3), (4, 5), (6, 7)]:
        own = ta_ < 4
        hcur = {}
        for tt in (ta_, tb__):
            si = nht[0] % 3
            nht[0] += 1
            (ha, hbuf) = hTs[si]
            ha3 = ha.rearrange("p (k t) -> p k t", k=KC)
            load(ha3, r32(hT_d[:, :, tt * 512:(tt + 1) * 512]).rearrange("k p t -> p k t"), hds2[si], hbuf, rbufs=[b_hTd])
            hcur[tt] = (ha3, hbuf)
        for hg in range(28):
            kind = kinds[hg // 2]
            if (not own) and kind in ("q", "rq", "rg"):
                continue
            wi = nw[0] % 2
            nw[0] += 1
            (wa, wb) = wts[wi]
            wa3 = wa.rearrange("p (k c) -> p k c", k=KC)
            load(wa3, r32(w_in[:, hg * 256:(hg + 1) * 256]).rearrange("(k p) c -> p k c", p=128), wds2[wi], wb)
            hb = (hg % 4) * 2
            for tt in (ta_, tb__):
                ha3, hbuf = hcur[tt]
                if kind in ("v", "rv"):
                    for sub in range(4):
                        pt, pb = psnext()
                        (sa, sbuf), sd = stg[nst[0] % 4], sds[nst[0] % 4]
                        nst[0] += 1
                        for kc in range(KC):
                            k.op("pe", lambda e, pt=pt, kc=kc, ha3=ha3, wa3=wa3, sub=sub: e.matmul(
                                pt[:, 0:256], lhsT=ha3[:, kc, sub * 128:(sub + 1) * 128], rhs=wa3[:, kc, :], start=(kc == 0), stop=(kc == KC - 1)),
                                reads=[hbuf, wb], writes=[pb])
                        tok0 = tt * 512 + sub * 128
                        if tok0 >= SO:
                            k.op("dve", lambda e, pt=pt, sa=sa: e.tensor_scalar(out=sa[:, 0:256], in0=pt[:, 0:256], scalar1=flag[:, 0:1], scalar2=None, op0=ALU.mult),
                                 reads=[pb, b_flag], writes=[sbuf])
                        else:
                            k.op("dve", lambda e, pt=pt, sa=sa: e.tensor_copy(out=sa[:, 0:256], in_=pt[:, 0:256]), reads=[pb], writes=[sbuf])
                        dst = v_d if kind == "v" else rv_d
                        c0 = (hg % 4) * 256
                        store(dst[tok0:tok0 + 128, c0:c0 + 256], sa[:, 0:256], sd, sbuf, wbufs=[dbuf(kind)])
                else:
                    for sub in range(2):
                        pt, pb = psnext()
                        (sa, sbuf), sd = stg[nst[0] % 4], sds[nst[0] % 4]
                        nst[0] += 1
                        for kc in range(KC):
                            k.op("pe", lambda e, pt=pt, kc=kc, ha3=ha3, wa3=wa3, sub=sub: e.matmul(
                                pt[:], lhsT=wa3[:, kc, sub * 128:(sub + 1) * 128], rhs=ha3[:, kc, :], start=(kc == 0), stop=(kc == KC - 1)),
                                reads=[hbuf, wb], writes=[pb])
                        head = hb + sub
                        tsl = slice(tt * 512, (tt + 1) * 512)
                        if kind == "q":
                            k.op("act", lambda e, pt=pt, sa=sa: e.activation(out=sa, in_=pt[:], func=AF.Copy, scale=float(128 ** -0.5)),
                                 reads=[pb], writes=[sbuf])
                            store(qT_d[head, :, tsl], sa, sd, sbuf, wbufs=[dbuf("q")])
                        elif kind == "k":
                            k.op("act", lambda e, pt=pt, sa=sa: e.copy(out=sa, in_=pt[:]), reads=[pb], writes=[sbuf])
                            store(kT_d[head, :, tsl], sa, sd, sbuf, wbufs=[dbuf("k")])
                        elif kind == "rg":
                            k.op("act", lambda e, pt=pt, sa=sa: e.activation(out=sa, in_=pt[:], func=AF.Silu), reads=[pb], writes=[sbuf])
                            store(rgT_d[head, :, tsl], sa, sd, sbuf, wbufs=[dbuf("rg")])
                        else:
                            k.op("act", lambda e, pt=pt: e.copy(out=ut, in_=pt[:]), reads=[pb], writes=[b_ut])
                            p2, pb2 = psnext()
                            k.op("pe", lambda e, p2=p2: e.matmul(p2[:], lhsT=prot_t, rhs=ut, start=True, stop=True),
                                 reads=[b_prot, b_ut], writes=[pb2])
                            k.op("dve", lambda e, tsl=tsl: e.tensor_tensor(out=t1, in0=f32(ut), in1=cs_t[:, tsl], op=ALU.mult),
                                 reads=[b_ut, b_cost], writes=[b_t1])
                            k.op("dve", lambda e, p2=p2, tsl=tsl: e.tensor_tensor(out=t2, in0=p2[:], in1=sn_t[:, tsl], op=ALU.mult),
                                 reads=[pb2, b_sint], writes=[b_t2])
                            k.op("pool", lambda e, sa=sa: e.tensor_tensor(out=sa, in0=t1, in1=t2, op=ALU.add), reads=[b_t1, b_t2], writes=[sbuf])
                            dst = rqT_d if kind == "rq" else rkT_d
                            store(dst[head, :, tsl], sa, sd, sbuf, wbufs=[dbuf(kind)])
    k.barrier()
    if stage <= 2:
        return finish(k, nc)

    ar.reset()
    qs = [ar.alloc(SO, F32R) for _ in range(2)]
    ks_ = [ar.alloc(SC, F32R) for _ in range(2)]
    vs = [ar.alloc(SC, F32R) for _ in range(2)]
    qkvd = [[k.dsem() for _ in range(3)] for _ in range(2)]
    ohs = [ar.alloc(SO) for _ in range(2)]
    ohd = [k.dsem() for _ in range(2)]
    NB = 4
    e_t = [ar.alloc(512) for _ in range(NB)]
    sp_t = [ar.alloc(512) for _ in range(NB)]
    G_t = [ar.alloc(512) for _ in range(NB)]
    t_t = [ar.alloc(512) for _ in range(NB)]
    a_t = [ar.alloc(512) for _ in range(NB)]
    aT_t = [ar.alloc(512, F32R) for _ in range(NB)]
    b_oTd = dbuf("oT")
    blocks = []
    nqt = 0
    for h in range(8):
        for qt in range(16):
            nk = 32 - qt
            nch = (nk + 3) // 4
            for c in range(nch):
                kt0 = qt + 4 * c
                ntile = min(4, 32 - kt0)
                blocks.append(dict(h=h, qt=qt, c=c, nch=nch, kt0=kt0, ntile=ntile, W=ntile * 128, i=len(blocks), nqt=nqt))
            nqt += 1
    hstate = {}

    def head_bufs(h):
        (qa, qb), (ka, kb_), (va, vb) = qs[h % 2], ks_[h % 2], vs[h % 2]
        va3 = va.rearrange("p (t e) -> p t e", t=32)
        return qa, qb, ka, kb_, va3, vb

    def stageA(B):
        h, qt, c, kt0, W, i = B["h"], B["qt"], B["c"], B["kt0"], B["W"], B["i"]
        qa, qb, ka, kb_, va3, vb = head_bufs(h)
        if qt == 0 and c == 0:
            d3 = qkvd[h % 2]
            load(qa, r32(qT_d[h]), d3[0], qb, rbufs=[dbuf("q")])
            load(ka, r32(kT_d[h]), d3[1], kb_, rbufs=[dbuf("k")])
            load(va3, r32(v_d[:, h * 128:(h + 1) * 128]).rearrange("(t p) e -> p t e", p=128), d3[2], vb, rbufs=[dbuf("v")])
        s_ = i % NB
        (ea, eb), (spa, spb) = e_t[s_], sp_t[s_]
        pz, pzb = PS[i % 3], PB[i % 3]
        k.op("pe", lambda e: e.matmul(pz[:, 0:W], lhsT=qa[:, qt * 128:(qt + 1) * 128], rhs=ka[:, kt0 * 128:kt0 * 128 + W], start=True, stop=True),
             reads=[qb, kb_], writes=[pzb])
        k.op("act", lambda e: e.activation(out=ea[:, 0:W], in_=pz[:, 0:W], func=AF.Exp), reads=[pzb], writes=[eb])
        k.op("act", lambda e: e.activation(out=spa[:, 0:W], in_=ea[:, 0:W], func=AF.Ln, bias=1.0), reads=[eb], writes=[spb])
        if c == 0:
            k.op("pool", lambda e: e.affine_select(out=spa[:, 0:128], in_=spa[:, 0:128], pattern=[[1, 128]],
                                                   compare_op=ALU.is_gt, fill=0.0, base=0, channel_multiplier=-1),
                 reads=[spb], writes=[spb])

    def stageB(B):
        c, W, i = B["c"], B["W"], B["i"]
        s_ = i % NB
        (spa, spb), (Ga, Gb), (ta, tb_) = sp_t[s_], G_t[s_], t_t[s_]
        pz, pzb = PS[i % 3], PB[i % 3]
        if c == 0:
            init, rd = 0.0, [spb]
        else:
            (Gp, Gpb) = G_t[(i - 1) % NB]
            init, rd = Gp[:, 511:512], [spb, Gpb]
        k.op("dve", lambda e: e.tensor_tensor_scan(out=Ga[:, 0:W], data0=spa[:, 0:W], data1=spa[:, 0:W], initial=init, op0=ALU.add, op1=ALU.bypass),
             reads=rd, writes=[Gb])
        k.op("dve", lambda e: e.tensor_tensor(out=ta[:, 0:W], in0=pz[:, 0:W], in1=Ga[:, 0:W], op=ALU.subtract), reads=[pzb, Gb], writes=[tb_])

    def cvars(B):
        i = B["i"]
        s_ = i % NB
        return t_t[s_], a_t[s_], aT_t[s_], (PS[3 + i % 2], PB[3 + i % 2])

    def stage3(B):
        c, W = B["c"], B["W"]
        (ta, tb_), (aa, ab), _, _ = cvars(B)
        k.op("act", lambda e: e.activation(out=aa[:, 0:W], in_=ta[:, 0:W], func=AF.Exp), reads=[tb_], writes=[ab])
        if c == 0:
            k.op("pool", lambda e: e.affine_select(out=aa[:, 0:128], in_=aa[:, 0:128], pattern=[[1, 128]],
                                                   compare_op=ALU.is_gt, fill=0.0, base=0, channel_multiplier=-1),
                 reads=[ab], writes=[ab])

    def stage4(B):
        _, (aa, ab), _, (pT, pTb) = cvars(B)
        for s in range(B["ntile"]):
            k.op("pe", lambda e, s=s: e.transpose(pT[:, s * 128:(s + 1) * 128], aa[:, s * 128:(s + 1) * 128], ident[:]),
                 reads=[ab, b_ident], writes=[pTb])

    def stage5(B):
        W, i = B["W"], B["i"]
        _, _, (aTa, aTb), (pT, pTb) = cvars(B)
        if i % 2 == 0:
            k.op("act", lambda e: e.copy(out=aTa[:, 0:W], in_=pT[:, 0:W]), reads=[pTb], writes=[aTb])
        else:
            k.op("dve", lambda e: e.tensor_copy(out=aTa[:, 0:W], in_=pT[:, 0:W]), reads=[pTb], writes=[aTb])

    def stage6(B):
        h, qt, c, nch, kt0, ntile = B["h"], B["qt"], B["c"], B["nch"], B["kt0"], B["ntile"]
        qa, qb, ka, kb_, va3, vb = head_bufs(h)
        _, _, (aTa, aTb), _ = cvars(B)
        po, pob = PS[5 + B["nqt"] % 2], PB[5 + B["nqt"] % 2]
        (oh, ohb) = ohs[h % 2]
        for s in range(ntile):
            first = (c == 0 and s == 0)
            last = (c == nch - 1) and (s == ntile - 1)
            k.op("pe", lambda e, s=s, first=first, last=last: e.matmul(po[:, 0:128], lhsT=va3[:, kt0 + s, :], rhs=aTa[:, s * 128:(s + 1) * 128],
                                                                        start=first, stop=last),
                 reads=[vb, aTb], writes=[pob])
        if c == nch - 1:
            k.op("act", lambda e: e.copy(out=oh[:, qt * 128:(qt + 1) * 128], in_=po[:, 0:128]), reads=[pob], writes=[ohb])
            if qt == 15:
                store(oT_d[h], oh, ohd[h % 2], ohb, wbufs=[b_oTd])

    nblocks = len(blocks)
    stages = [stageA, stageB, stage3, stage4, stage5, stage6]
    for s in range(nblocks + len(stages) - 1):
        for d, fn in enumerate(stages):
            if 0 <= s - d < nblocks:
                fn(blocks[s - d])
    k.pool = list(range(8))
    k.barrier()
    if stage <= 3:
        return finish(k, nc)

    ar.reset()
    rqs = [ar.alloc(SO, F32R) for _ in range(2)]
    rks = [ar.alloc(SC, F32R) for _ in range(2)]
    rvs = [ar.alloc(SC, F32R) for _ in range(2)]
    rgs = [ar.alloc(SO) for _ in range(2)]
    rd4 = [[k.dsem() for _ in range(4)] for _ in range(2)]
    oraw, b_oraw = ar.alloc(SO)
    ofin = [ar.alloc(SO) for _ in range(2)]
    ofd = [k.dsem() for _ in range(2)]
    dm_t, b_dm = ar.alloc(8 * 128)
    qd_t, b_qd = ar.alloc(8 * 128)
    kd_t, b_kd = ar.alloc(8)
    cd_t, b_cd = ar.alloc(8)
    pd_t, b_pd = ar.alloc(8 * 16)
    gg_t, b_gg = ar.alloc(8)
    onesr, b_onesr = ar.alloc(128, F32R)
    load(dm_t, dmatT, k.dsem(), b_dm)
    load(qd_t, qdec, k.dsem(), b_qd)
    load(kd_t, kdec, k.dsem(), b_kd)
    load(cd_t, cdec, k.dsem(), b_cd)
    load(pd_t, pdec, k.dsem(), b_pd)
    load(gg_t, ggn, k.dsem(), b_gg)
    load(onesr, r32(ones_d), k.dsem(), b_onesr)
    Rs = [ar.alloc(128, F32R) for _ in range(2)]
    ktk = [ar.alloc(128, F32R) for _ in range(3)]
    sm_t = [ar.alloc(128, F32R) for _ in range(2)]
    qdx = [ar.alloc(128, F32R) for _ in range(2)]
    sq_t, b_sq = ar.alloc(512, F32R)
    rs_t, b_rs = ar.alloc(512)
    on_t, b_on = ar.alloc(512)
    nk_ = [0]
    k.pool = list(range(7))
    for h in range(8):
        (qa, qb), (ka, kb_), (va, vb), (ga, gb) = rqs[h % 2], rks[h % 2], rvs[h % 2], rgs[h % 2]
        d4 = rd4[h % 2]
        load(qa, r32(rqT_d[h]), d4[0], qb, rbufs=[dbuf("rq")])
        load(ka, r32(rkT_d[h]), d4[1], kb_, rbufs=[dbuf("rk")])
        va3 = va.rearrange("p (t e) -> p t e", t=32)
        load(va3, r32(rv_d[:, h * 128:(h + 1) * 128]).rearrange("(t p) e -> p t e", p=128), d4[2], vb, rbufs=[dbuf("rv")])
        load(ga, rgT_d[h], d4[3], gb, rbufs=[dbuf("rg")])
        pR, pRb = PS[7], PB[7]
        for t in range(16):
            (kt, ktb) = ktk[nk_[0] % 3]
            nk_[0] += 1
            pt, pb = psnext()
            k.op("pe", lambda e, pt=pt, ka=ka, t=t: e.transpose(pt[:, 0:128], f32(ka)[:, (16 + t) * 128:(17 + t) * 128], ident[:]),
                 reads=[kb_, b_ident], writes=[pb])
            k.op("dve", lambda e, kt=kt, pt=pt, h=h, t=t: e.tensor_scalar(out=kt, in0=pt[:, 0:128], scalar1=pd_t[:, h * 16 + t:h * 16 + t + 1],
                                                                         scalar2=None, op0=ALU.mult), reads=[pb, b_pd], writes=[ktb])
            k.op("pe", lambda e, pR=pR, kt=kt, va3=va3, t=t: e.matmul(pR[:, 0:128], lhsT=kt, rhs=va3[:, 16 + t, :], start=(t == 0), stop=(t == 15)),
                 reads=[ktb, vb], writes=[pRb])
        ri = 0
        (R, Rb) = Rs[ri]
        k.op("dve", lambda e, R=R, pR=pR: e.tensor_scalar(out=R, in0=pR[:, 0:128], scalar1=flag[:, 0:1], scalar2=None, op0=ALU.mult),
             reads=[pRb, b_flag], writes=[Rb])
        for cr in range(15, -1, -1):
            csl = slice(cr * 128, (cr + 1) * 128)
            (sm, smb) = sm_t[cr % 2]
            (qx, qxb) = qdx[cr % 2]
            pS, pSb = psnext()
            k.op("pe", lambda e, pS=pS, ka=ka, qa=qa, csl=csl: e.matmul(pS[:, 0:128], lhsT=ka[:, csl], rhs=qa[:, csl], start=True, stop=True),
                 reads=[kb_, qb], writes=[pSb])
            k.op("dve", lambda e, sm=sm, pS=pS, h=h: e.tensor_tensor(out=sm, in0=pS[:, 0:128], in1=dm_t[:, h * 128:(h + 1) * 128], op=ALU.mult),
                 reads=[pSb, b_dm], writes=[smb])
            k.op("pool", lambda e, qx=qx, qa=qa, csl=csl, h=h: e.tensor_tensor(out=qx, in0=f32(qa)[:, csl], in1=qd_t[:, h * 128:(h + 1) * 128], op=ALU.mult),
                 reads=[qb, b_qd], writes=[qxb])
            pO, pOb = psnext()
            k.op("pe", lambda e, pO=pO, va3=va3, sm=sm, cr=cr: e.matmul(pO[:, 0:128], lhsT=va3[:, cr, :], rhs=sm, start=True, stop=False),
                 reads=[vb, smb], writes=[pOb])
            k.op("pe", lambda e, pO=pO, R=R, qx=qx: e.matmul(pO[:, 0:128], lhsT=R, rhs=qx, start=False, stop=True),
                 reads=[Rb, qxb], writes=[pOb])
            k.op("act", lambda e, pO=pO, csl=csl: e.copy(out=oraw[:, csl], in_=pO[:, 0:128]), reads=[pOb], writes=[b_oraw])
            if cr > 0:
                (kt, ktb) = ktk[nk_[0] % 3]
                nk_[0] += 1
                pt, pb = psnext()
                k.op("pe", lambda e, pt=pt, ka=ka, csl=csl: e.transpose(pt[:, 0:128], f32(ka)[:, csl], ident[:]), reads=[kb_, b_ident], writes=[pb])
                k.op("dve", lambda e, kt=kt, pt=pt, h=h: e.tensor_scalar(out=kt, in0=pt[:, 0:128], scalar1=kd_t[:, h:h + 1], scalar2=None, op0=ALU.mult),
                     reads=[pb, b_kd], writes=[ktb])
                pK, pKb = psnext()
                k.op("pe", lambda e, pK=pK, kt=kt, va3=va3, cr=cr: e.matmul(pK[:, 0:128], lhsT=kt, rhs=va3[:, cr, :], start=True, stop=True),
                     reads=[ktb, vb], writes=[pKb])
                ri ^= 1
                (Rn, Rnb) = Rs[ri]
                k.op("dve", lambda e, Rn=Rn, R=R, pK=pK, h=h: e.scalar_tensor_tensor(out=Rn, in0=f32(R), scalar=cd_t[:, h:h + 1], in1=pK[:, 0:128],
                                                                                    op0=ALU.mult, op1=ALU.add),
                     reads=[Rb, pKb, b_cd], writes=[Rnb])
                R, Rb = Rn, Rnb
        (of, ofb) = ofin[h % 2]
        for sl in range(4):
            ssl = slice(sl * 512, (sl + 1) * 512)
            k.op("dve", lambda e, ssl=ssl: e.tensor_tensor(out=sq_t, in0=oraw[:, ssl], in1=oraw[:, ssl], op=ALU.mult), reads=[b_oraw], writes=[b_sq])
            pN, pNb = psnext()
            k.op("pe", lambda e, pN=pN: e.matmul(pN[:], lhsT=onesr, rhs=sq_t, start=True, stop=True), reads=[b_onesr, b_sq], writes=[pNb])
            k.op("dve", lambda e, pN=pN: e.tensor_scalar(out=rs_t, in0=pN[:], scalar1=1.0 / 128, scalar2=EPS, op0=ALU.mult, op1=ALU.add),
                 reads=[pNb], writes=[b_rs])
            k.op("act", lambda e: e.activation(out=rs_t, in_=rs_t, func=AF.Sqrt), reads=[b_rs], writes=[b_rs])
            k.op("dve", lambda e: e.reciprocal(out=rs_t, in_=rs_t), reads=[b_rs], writes=[b_rs])
            k.op("dve", lambda e, ssl=ssl: e.tensor_tensor(out=on_t, in0=oraw[:, ssl], in1=rs_t, op=ALU.mult), reads=[b_oraw, b_rs], writes=[b_on])
            k.op("dve", lambda e, of=of, ga=ga, ssl=ssl, h=h: e.scalar_tensor_tensor(out=of[:, ssl], in0=on_t, scalar=gg_t[:, h:h + 1], in1=ga[:, ssl],
                                                                                    op0=ALU.mult, op1=ALU.mult),
                 reads=[b_on, b_gg, gb], writes=[ofb])
        store(oT_d[8 + h], of, ofd[h % 2], ofb, wbufs=[b_oTd])
    k.pool = list(range(8))
    k.barrier()
    if stage <= 4:
        return finish(k, nc)

    ar.reset()
    wos = [ar.alloc(KC * 512, F32R) for _ in range(2)]
    wod = [k.dsem() for _ in range(2)]
    g1b = [ar.alloc(512) for _ in range(2)]
    g1d = [k.dsem() for _ in range(2)]
    ots = [ar.alloc(KC * 128, F32R) for _ in range(2)]
    otd = [k.dsem() for _ in range(2)]
    xsl = [ar.alloc(512) for _ in range(2)]
    xsd = [k.dsem() for _ in range(2)]
    tm_t, b_tm = ar.alloc(512)
    x1s = [ar.alloc(512) for _ in range(2)]
    x1sd = [k.dsem() for _ in range(2)]
    b_x1d = dbuf("x1")
    n5 = 0
    for cg in range(4):
        (wa, wb), wd = wos[cg % 2], wod[cg % 2]
        wa3 = wa.rearrange("p (k c) -> p k c", k=KC)
        load(wa3, r32(w_out[:, cg * 512:(cg + 1) * 512]).rearrange("(k p) c -> p k c", p=128), wd, wb)
        (ga, gb) = g1b[cg % 2]
        load(ga, modb_d[:, 2 * D + cg * 512:2 * D + (cg + 1) * 512], g1d[cg % 2], gb, rbufs=[b_modb])
        for t in range(16):
            (oa, ob), od = ots[n5 % 2], otd[n5 % 2]
            (xa, xb), xd = xsl[n5 % 2], xsd[n5 % 2]
            (x1a, x1b), x1d_ = x1s[n5 % 2], x1sd[n5 % 2]
            n5 += 1
            oa3 = oa.rearrange("p (k t) -> p k t", k=KC)
            load(oa3, r32(oT_d[:, :, t * 128:(t + 1) * 128]).rearrange("k p t -> p k t"), od, ob, rbufs=[b_oTd])
            load(xa, xr[t * 128:(t + 1) * 128, cg * 512:(cg + 1) * 512], xd, xb)
            pt, pb = psnext()
            for kc in range(KC):
                k.op("pe", lambda e, pt=pt, oa3=oa3, wa3=wa3, kc=kc: e.matmul(pt[:], lhsT=oa3[:, kc, :], rhs=wa3[:, kc, :], start=(kc == 0), stop=(kc == KC - 1)),
                     reads=[ob, wb], writes=[pb])
            k.op("dve", lambda e, pt=pt, ga=ga: e.tensor_tensor(out=tm_t, in0=pt[:], in1=ga, op=ALU.mult), reads=[pb, gb], writes=[b_tm])
            k.op("pool", lambda e, x1a=x1a, xa=xa: e.tensor_tensor(out=x1a, in0=tm_t, in1=xa, op=ALU.add), reads=[b_tm, xb], writes=[x1b])
            store(x1_d[t * 128:(t + 1) * 128, cg * 512:(cg + 1) * 512], x1a, x1d_, x1b, wbufs=[b_x1d])
    k.barrier()
    if stage <= 5:
        return finish(k, nc)

    ar.reset()
    wr_t, b_wr = ar.alloc(KC * NE)
    rb_t, b_rb = ar.alloc(NE)
    wr3 = wr_t.rearrange("p (k c) -> p k c", k=KC)
    load(wr3, w_router.rearrange("(k p) c -> p k c", p=128), k.dsem(), b_wr)
    load(rb_t, rbias, k.dsem(), b_rb)
    s_t, b_s = ar.alloc(NE)
    ch_t, b_ch = ar.alloc(NE)
    top_t, b_top = ar.alloc(64)
    gs_t, b_gs = ar.alloc(8)
    g8_t, b_g8 = ar.alloc(8)
    gm_t, b_gm = ar.alloc(8)
    pen_t, b_pen = ar.alloc(8)
    cm_t, b_cm = ar.alloc(NE)
    e8_t, b_e8 = ar.alloc(8)
    em_t, b_em = ar.alloc(NE)
    gsel_t, b_gsel = ar.alloc(NE)
    den_t, b_den = ar.alloc(2)
    b_h2Td = dbuf("h2T")
    k.op("pool", lambda e: e.memset(gw[:], 1.0), writes=[b_gw])

    def router(t, hF3, b_hF, bank):
        pl, plb = bank
        for kc in range(KC):
            k.op("pe", lambda e, kc=kc: e.matmul(pl[:, 0:NE], lhsT=hF3[:, kc, :], rhs=wr3[:, kc, :], start=(kc == 0), stop=(kc == KC - 1)),
                 reads=[b_hF, b_wr], writes=[plb])
        k.op("act", lambda e: e.activation(out=s_t, in_=pl[:, 0:NE], func=AF.Sigmoid), reads=[plb], writes=[b_s])
        k.op("dve", lambda e: e.tensor_tensor(out=ch_t, in0=s_t, in1=rb_t, op=ALU.add), reads=[b_s, b_rb], writes=[b_ch])
        for g in range(8):
            k.op("dve", lambda e, g=g: e.max(out=top_t[:, g * 8:(g + 1) * 8], in_=ch_t[:, g * 8:(g + 1) * 8]), reads=[b_ch], writes=[b_top])
        top3 = top_t.rearrange("p (g j) -> p g j", g=8)
        k.op("dve", lambda e: e.tensor_tensor(out=gs_t.rearrange("p (g o) -> p g o", o=1), in0=top3[:, :, 0:1], in1=top3[:, :, 1:2], op=ALU.add),
             reads=[b_top], writes=[b_gs])
        k.op("dve", lambda e: e.max(out=g8_t, in_=gs_t), reads=[b_gs], writes=[b_g8])
        k.op("dve", lambda e: e.tensor_scalar(out=gm_t, in0=gs_t, scalar1=g8_t[:, 3:4], scalar2=None, op0=ALU.is_ge), reads=[b_gs, b_g8], writes=[b_gm])
        k.op("dve", lambda e: e.tensor_scalar(out=pen_t, in0=gm_t, scalar1=1e9, scalar2=-1e9, op0=ALU.mult, op1=ALU.add), reads=[b_gm], writes=[b_pen])
        cm3 = cm_t.rearrange("p (g j) -> p g j", g=8)
        ch3 = ch_t.rearrange("p (g j) -> p g j", g=8)
        k.op("dve", lambda e: e.tensor_tensor(out=cm3, in0=ch3, in1=gm_t.unsqueeze(2).broadcast_to([128, 8, 8]), op=ALU.mult),
             reads=[b_ch, b_gm], writes=[b_cm])
        k.op("dve", lambda e: e.tensor_tensor(out=cm3, in0=cm3, in1=pen_t.unsqueeze(2).broadcast_to([128, 8, 8]), op=ALU.add),
             reads=[b_cm, b_pen], writes=[b_cm])
        k.op("dve", lambda e: e.max(out=e8_t, in_=cm_t), reads=[b_cm], writes=[b_e8])
        k.op("dve", lambda e: e.tensor_scalar(out=em_t, in0=cm_t, scalar1=e8_t[:, 7:8], scalar2=None, op0=ALU.is_ge), reads=[b_cm, b_e8], writes=[b_em])
        k.op("dve", lambda e: e.tensor_tensor(out=gsel_t, in0=s_t, in1=em_t, op=ALU.mult), reads=[b_s, b_em], writes=[b_gsel])
        k.op("dve", lambda e: e.reduce_sum(out=den_t[:, 0:1], in_=gsel_t, axis=mybir.AxisListType.X), reads=[b_gsel], writes=[b_den])
        k.op("dve", lambda e: e.reciprocal(out=den_t[:, 1:2], in_=den_t[:, 0:1]), reads=[b_den], writes=[b_den])
        k.op("dve", lambda e: e.tensor_scalar(out=gw[:, t * 65:t * 65 + NE], in0=gsel_t, scalar1=den_t[:, 1:2], scalar2=2.5, op0=ALU.mult, op1=ALU.mult),
             reads=[b_gsel, b_den], writes=[b_gw])

    norm_phase(16, lambda t: x1_d[t * 128:(t + 1) * 128, :], [b_x1d], h2T_d, b_h2Td, 2 * KC, router=router)
    if debug:
        gw_dbg = nc.dram_tensor("gw_dbg", [128, 16 * 65], F32, kind="ExternalOutput").ap()
        store(gw_dbg, gw[:], k.dsem(), b_gw)
    k.barrier()
    if stage <= 6:
        return finish(k, nc)

    ar.reset()
    h2, b_h2 = ar.alloc(KC * 512, F32R)
    h2d = k.dsem()
    yacc, b_yacc = ar.alloc(4 * D)
    yd = k.dsem()
    acts = [ar.alloc(4 * 512, F32R) for _ in range(2)]
    NSLOT = 6
    slots = [ar.alloc(4096, F32R) for _ in range(NSLOT)]
    sld = [k.dsem() for _ in range(NSLOT)]
    nsl = [0]
    GB = [(PS[i], PB[i]) for i in range(4)]
    UB = [(PS[i], PB[i]) for i in range(4, 8)]
    b_yd = dbuf("y")
    h23 = h2.rearrange("p (k t) -> p k t", k=KC)
    yacc3 = yacc.rearrange("p (t d) -> p t d", t=4)

    def wsrc(e, which):
        if e < NE:
            return {"g": w_gate[e], "u": w_up[e], "d": w_down[e]}[which]
        return {"g": ws_gate, "u": ws_up, "d": ws_down}[which]

    def load_gu(e, which):
        res = []
        src = r32(wsrc(e, which)).rearrange("(k p) c -> p k c", p=128)
        base = 0 if which == "g" else 2
        for hf in range(2):
            i = base + hf
            (sa, sbuf) = slots[i]
            sa3 = sa.rearrange("p (k c) -> p k c", k=8)
            load(sa3, src[:, hf * 8:(hf + 1) * 8, :], sld[i], sbuf)
            res.append((sa3, sbuf))
        return res

    def load_d(e):
        res = []
        src = r32(wsrc(e, "d")).rearrange("(k p) c -> p k c", p=128)
        for hf in range(2):
            i = 4 + hf
            (sa, sbuf) = slots[i]
            sa3 = sa.rearrange("p (k c) -> p k c", k=4)
            load(sa3, src[:, :, hf * 1024:(hf + 1) * 1024], sld[i], sbuf)
            res.append((sa3, sbuf))
        return res

    def gu_matmuls(halves, banks):
        for hf in range(2):
            sa3, sbuf = halves[hf]
            for k8 in range(8):
                kc = hf * 8 + k8
                for fc in range(4):
                    pt, pb = banks[fc]
                    k.op("pe", lambda e, pt=pt, sa3=sa3, k8=k8, fc=fc, kc=kc: e.matmul(
                        pt[:], lhsT=sa3[:, k8, fc * 128:(fc + 1) * 128], rhs=h23[:, kc, :], start=(kc == 0), stop=(kc == KC - 1)),
                        reads=[sbuf, b_h2], writes=[pb])

    def down(e, p, dh, act3, actb, first):
        nd = 0
        for hf in range(2):
            sa3, sbuf = dh[hf]
            for tb in range(4):
                for dc2 in range(2):
                    pt, pb = GB[nd % 4]
                    nd += 1
                    for fc in range(4):
                        k.op("pe", lambda e_, pt=pt, act3=act3, sa3=sa3, fc=fc, tb=tb, dc2=dc2: e_.matmul(
                            pt[:], lhsT=act3[:, fc, tb * 128:(tb + 1) * 128], rhs=sa3[:, fc, dc2 * 512:(dc2 + 1) * 512],
                            start=(fc == 0), stop=(fc == 3)), reads=[actb, sbuf], writes=[pb])
                    dsl = slice(hf * 1024 + dc2 * 512, hf * 1024 + (dc2 + 1) * 512)
                    tile = p * 4 + tb
                    gcol_ = gw[:, tile * 65 + e:tile * 65 + e + 1]
                    if first:
                        k.op("dve", lambda e_, pt=pt, tb=tb, dsl=dsl, gcol_=gcol_: e_.tensor_scalar(
                            out=yacc3[:, tb, dsl], in0=pt[:], scalar1=gcol_, scalar2=None, op0=ALU.mult),
                            reads=[pb, b_gw], writes=[b_yacc])
                    else:
                        k.op("dve", lambda e_, pt=pt, tb=tb, dsl=dsl, gcol_=gcol_: e_.scalar_tensor_tensor(
                            out=yacc3[:, tb, dsl], in0=pt[:], scalar=gcol_, in1=yacc3[:, tb, dsl], op0=ALU.mult, op1=ALU.add),
                            reads=[pb, b_gw, b_yacc], writes=[b_yacc])

    NEXP = NE + 1
    for p in range(4):
        load(h23, r32(h2T_d[:, :, p * 512:(p + 1) * 512]).rearrange("k p t -> p k t"), h2d, b_h2, rbufs=[b_h2Td])
        pend = None
        gh = load_gu(0, "g")
        uh = load_gu(0, "u")
        for e in range(NEXP):
            gu_matmuls(gh, GB)
            (aa, ab) = acts[e % 2]
            for fc in range(4):
                pt, pb = GB[fc]
                k.op("act", lambda e_, pt=pt, fc=fc, aa=aa: e_.activation(out=aa[:, fc * 512:(fc + 1) * 512], in_=pt[:], func=AF.Silu),
                     reads=[pb], writes=[ab])
            gu_matmuls(uh, UB)
            for fc in range(4):
                pt, pb = UB[fc]
                k.op("dve", lambda e_, pt=pt, fc=fc, aa=aa: e_.tensor_tensor(out=aa[:, fc * 512:(fc + 1) * 512], in0=pt[:], in1=f32(aa)[:, fc * 512:(fc + 1) * 512], op=ALU.mult),
                     reads=[pb, ab], writes=[ab])
            if e + 1 < NEXP:
                gh = load_gu(e + 1, "g")
                uh = load_gu(e + 1, "u")
            if pend is not None:
                down(*pend)
            dh = load_d(e)
            pend = (e, p, dh, aa.rearrange("p (f t) -> p f t", f=4), ab, e == 0)
        down(*pend)
        for tb in range(4):
            tile = p * 4 + tb
            store(y_d[tile * 128:(tile + 1) * 128, :], yacc3[:, tb, :], yd, b_yacc, wbufs=[b_yd])
    k.barrier()
    if stage <= 7:
        return finish(k, nc)

    ar.reset()
    g2b, b_g2b = ar.alloc(D)
    afb, b_afb = ar.alloc(D)
    shfb, b_shfb = ar.alloc(D)
    yts = [ar.alloc(D) for _ in range(2)]
    ytd = [k.dsem() for _ in range(2)]
    x1t = [ar.alloc(D) for _ in range(2)]
    x1td = [k.dsem() for _ in range(2)]
    otd_ = [k.dsem() for _ in range(2)]
    stat, b_stat = ar.alloc(4)
    load(g2b, modb_d[:, 5 * D:6 * D], k.dsem(), b_g2b, rbufs=[b_modb])
    load(shfb, modb_d[:, 6 * D:7 * D], k.dsem(), b_shfb, rbufs=[b_modb])
    load(afb, modb_d[:, 7 * D:8 * D], k.dsem(), b_afb, rbufs=[b_modb])
    gfb, b_gfb = yts[1]
    load(gfb, gfrow.partition_broadcast(128), ytd[1], b_gfb)
    k.op("dve", lambda e: e.scalar_tensor_tensor(out=afb, in0=afb, scalar=1.0, in1=gfb, op0=ALU.add, op1=ALU.mult),
         reads=[b_afb, b_gfb], writes=[b_afb])
    def f1(t):
        (ya, yb), (xa, xb) = yts[t % 2], x1t[t % 2]
        load(ya, y_d[t * 128:(t + 1) * 128, :], ytd[t % 2], yb, rbufs=[b_yd])
        load(xa, x1_d[t * 128:(t + 1) * 128, :], x1td[t % 2], xb, rbufs=[b_x1d])
        k.op("pool", lambda e: e.tensor_tensor(out=ya, in0=ya, in1=g2b, op=ALU.mult), reads=[yb, b_g2b], writes=[yb])

    def f2(t):
        (ya, yb), (xa, xb) = yts[t % 2], x1t[t % 2]
        k.op("dve", lambda e: e.tensor_tensor(out=ya, in0=ya, in1=xa, op=ALU.add), reads=[yb, xb], writes=[yb])
        k.op("dve", lambda e: e.scalar_tensor_tensor(out=xa, in0=ya, scalar=1.0, in1=ya, op0=ALU.mult, op1=ALU.mult, accum_out=stat[:, 0:1]),
             reads=[yb], writes=[xb, b_stat])
        k.op("dve", lambda e: e.tensor_scalar(out=stat[:, 1:2], in0=stat[:, 0:1], scalar1=1.0 / D, scalar2=EPS, op0=ALU.mult, op1=ALU.add),
             reads=[b_stat], writes=[b_stat])
        k.op("act", lambda e: e.activation(out=stat[:, 2:3], in_=stat[:, 1:2], func=AF.Sqrt), reads=[b_stat], writes=[b_stat])
        k.op("dve", lambda e: e.reciprocal(out=stat[:, 3:4], in_=stat[:, 2:3]), reads=[b_stat], writes=[b_stat])

    def f3(t):
        (ya, yb), (xa, xb) = yts[t % 2], x1t[t % 2]
        k.op("dve", lambda e: e.scalar_tensor_tensor(out=xa, in0=ya, scalar=stat[:, 3:4], in1=afb, op0=ALU.mult, op1=ALU.mult),
             reads=[yb, b_stat, b_afb], writes=[xb])
        k.op("pool", lambda e: e.tensor_tensor(out=xa, in0=xa, in1=shfb, op=ALU.add), reads=[xb, b_shfb], writes=[xb])
        last_store[0] = store(out_d[t * 128:(t + 1) * 128, :], xa, otd_[t % 2], xb)

    pipeline(list(range(16)), [f1, f2, f3])
    k.barrier()
    return finish(k, nc)


def finish(k, nc):
    k.barrier()
    k.build()
    return nc


def _const_tables(hf):
    H, C = 8, 128
    scale = 128.0 ** -0.5
    r = np.arange(SC)
    if hf == 1:
        pos = (SC - 1 - r).astype(np.float64)
    else:
        pos = np.where(r < SO, SO - 1 - r, 0).astype(np.float64)
    inv_freq = (10000.0 ** (-np.arange(0, 128, 2, dtype=np.float32) / 128)).astype(np.float32)
    ang = (pos.astype(np.float32)[None, :] * np.tile(inv_freq, 2)[:, None]).astype(np.float32)
    cosT = np.cos(ang).astype(np.float32)
    sinT = np.sin(ang).astype(np.float32)
    prot = np.zeros((128, 128), np.float32)
    for m in range(64):
        prot[m + 64, m] = -1.0
        prot[m, m + 64] = 1.0
    log_g = np.log(1.0 - np.exp2(-5.0 - np.arange(H, dtype=np.float64)))
    ip = np.arange(C, dtype=np.float64)
    dmatT = np.zeros((128, H, 128), np.float64)
    qdec = np.zeros((128, H, 128), np.float64)
    for h in range(H):
        diff = ip[:, None] - ip[None, :]
        dmatT[:, h, :] = np.where(diff >= 0, np.exp(np.where(diff >= 0, diff, 0) * log_g[h]), 0.0) * scale
        qdec[:, h, :] = np.exp((128.0 - ip)[None, :] * log_g[h])
    kdec = np.exp(ip[:, None] * log_g[None, :]) * scale
    cdec = np.tile(np.exp(C * log_g)[None, :], (128, 1))
    pd = np.zeros((128, H, 16), np.float64)
    for t in range(16):
        pd[:, :, t] = np.exp((t * 128 + ip)[:, None] * log_g[None, :]) * scale
    return dict(cosT=cosT, sinT=sinT, prot=prot,
                dmatT=dmatT.reshape(128, -1).astype(np.float32), qdec=qdec.reshape(128, -1).astype(np.float32),
                kdec=kdec.astype(np.float32), cdec=cdec.astype(np.float32), pdec=pd.reshape(128, -1).astype(np.float32))


def make_in_maps(x, c, w_ada, b_ada, norm1_g, w_in, ret_gn_g, w_out, norm2_g, w_router, router_bias,
                 w_gate, w_up, w_down, ws_gate, ws_up, ws_down, w_ada_final, b_ada_final, norm_f_g):
    f = lambda a: np.ascontiguousarray(np.asarray(a, dtype=np.float32))
    col = lambda v: f(np.asarray(v).reshape(-1, 128).T)
    shared = dict(
        w_ada=f(w_ada[0]), b_ada=f(b_ada[0]).reshape(1, -1), w_adaf=f(w_ada_final), b_adaf=f(b_ada_final).reshape(1, -1),
        g1col=col(norm1_g[0]), g2col=col(norm2_g[0]), gfrow=f(norm_f_g).reshape(1, -1), ggn=col(ret_gn_g[0]),
        w_in=f(w_in[0]), w_out=f(w_out[0]), w_router=f(w_router[0]),
        rbias=f(np.broadcast_to(np.asarray(router_bias[0]).reshape(1, -1), (128, NE))),
        w_gate=f(w_gate[0]), w_up=f(w_up[0]), w_down=f(w_down[0]),
        ws_gate=f(ws_gate[0]), ws_up=f(ws_up[0]), ws_down=f(ws_down[0]),
        ident=np.eye(128, dtype=np.float32), ones=np.ones((128, 128), np.float32),
    )
    tabs = [_const_tables(0), _const_tables(1)]
    x = np.asarray(x)
    in_maps = []
    for core in range(8):
        b, hf = core // 2, core % 2
        if hf == 1:
            xrv = x[b, ::-1]
        else:
            own = x[b, SO - 1::-1]
            xrv = np.concatenate([own, own], axis=0)
        m = dict(shared)
        m.update(tabs[hf])
        m["xr"] = f(xrv)
        m["flagc"] = np.full((128, 1), float(hf), np.float32)
        m["ccol"] = col(np.asarray(c)[b])
        in_maps.append(m)
    return in_maps


def kernel(**inputs):
    in_maps = make_in_maps(**inputs)
    nc = build_program()
    res = run_bass_kernel_spmd(nc, in_maps, core_ids=list(range(8)))
    out = np.zeros((4, 4096, D), np.float32)
    for core in range(8):
        b, hf = core // 2, core % 2
        o = np.asarray(res.results[core]["out"])
        out[b, hf * SO:(hf + 1) * SO] = o[::-1]
    return out
```

```python
import contextlib
import numpy as np
import concourse.bass as bass
import concourse.mybir as mybir
from concourse.bass_utils import run_bass_kernel_spmd

F32 = mybir.dt.float32
F32R = mybir.dt.float32r
AF = mybir.ActivationFunctionType
ALU = mybir.AluOpType

EPOCH = 30000
D = 2048
KC = 16
SO = 2048
SC = 4096
NE = 64
FF = 512
EPS = 1e-6
ARENA_R = 36864
ARENA_F = 14976


class Buf:
    __slots__ = ("name", "w", "r")

    def __init__(self, name=""):
        self.name = name
        self.w = None
        self.r = []


class DSem:
    def __init__(self, key):
        self.key = key
        self.count = 0


class KB:
    ENGS = ("pe", "act", "dve", "pool", "sp")

    def __init__(self):
        self.nc = bass.Bass("TRN2", target_bir_lowering=False)
        self.nc.dge_precook = False
        self.stack = contextlib.ExitStack()
        self.streams = {e: [] for e in self.ENGS}
        self.seq = {e: 0 for e in self.ENGS}
        self.waited = {e: {} for e in self.ENGS}
        self.semkeys = []
        self.dsems = []
        self.nbuf = 0
        self.rr = 0
        self.free_dsems = []
        self.pool = list(range(8))

    def sb(self, name, shape, dtype=F32):
        return self.stack.enter_context(self.nc.sbuf_tensor(name, list(shape), dtype))

    def ps(self, name, shape, dtype=F32):
        return self.stack.enter_context(self.nc.psum_tensor(name, list(shape), dtype))

    def dsem(self):
        if self.free_dsems:
            return self.free_dsems.pop()
        k = "d%d" % (len(self.dsems) + 1)
        self.semkeys.append(k)
        d = DSem(k)
        self.dsems.append(d)
        return d

    def buf(self, name=""):
        self.nbuf += 1
        return Buf(name or "b%d" % self.nbuf)

    def _need(self, eng, tok, waits):
        if tok is None:
            return
        key, val = tok
        if self.waited[eng].get(key, 0) >= val:
            return
        if eng == "pe" and key.startswith("pe@"):
            return
        waits[key] = max(waits.get(key, 0), val)

    def _deps(self, eng, reads, writes, own_dsem=None):
        waits = {}
        for b in reads:
            self._need(eng, b.w, waits)
        for b in writes:
            if not (own_dsem is not None and b.w is not None and b.w[0] == own_dsem.key):
                self._need(eng, b.w, waits)
            for t in b.r:
                self._need(eng, t, waits)
        for k, v in waits.items():
            self.waited[eng][k] = v
        return sorted(waits.items())

    def _mark(self, tok, reads, writes):
        for b in reads:
            b.r.append(tok)
        for b in writes:
            b.w = tok
            b.r = []

    def _engtok(self, eng):
        s = self.seq[eng]
        if s == 0:
            return None
        ep = (s - 1) // EPOCH
        return ("%s@%d" % (eng, ep), s - ep * EPOCH)

    def op(self, eng, fn, reads=(), writes=()):
        waits = self._deps(eng, reads, writes)
        self.seq[eng] += 1
        tok = self._engtok(eng)
        if tok[0] not in self.semkeys:
            self.semkeys.append(tok[0])
        self.streams[eng].append((waits, fn, tok[0], 1))
        self._mark(tok, reads, writes)
        return tok

    def dma(self, q, out, in_, dsem, reads=(), writes=(), **kw):
        waits = self._deps(q, reads, writes, own_dsem=dsem)
        dsem.count += 16
        tok = (dsem.key, dsem.count)
        self.streams[q].append((waits, lambda e: e.dma_start(out=out, in_=in_, **kw), dsem.key, 16))
        self._mark(tok, reads, writes)
        return tok

    def wait_tok(self, eng, tok):
        waits = {}
        self._need(eng, tok, waits)
        for k, v in waits.items():
            self.waited[eng][k] = v
        if waits:
            self.streams[eng].append((sorted(waits.items()), None, None, 0))

    def barrier(self):
        toks = [self._engtok(e) for e in self.ENGS]
        toks += [(d.key, d.count) for d in self.dsems if d.count > 0]
        for e in self.ENGS:
            for t in toks:
                self.wait_tok(e, t)
        self.free_dsems = list(self.dsems)

    def build(self):
        nc = self.nc
        sems = {}
        for k in self.semkeys:
            sems[k] = self.stack.enter_context(nc.semaphore(k.replace("@", "_")))
        streams = self.streams

        def replay(name):
            def f(e):
                for waits, fn, key, amt in streams[name]:
                    for wk, wv in waits:
                        e.wait_ge(sems[wk], wv)
                    if fn is not None:
                        fn(e).then_inc(sems[key], amt)
            return f

        with nc.Block() as block:
            block.tensor(replay("pe"))
            block.scalar(replay("act"))
            block.vector(replay("dve"))
            block.gpsimd(replay("pool"))
            block.sync(replay("sp"))
        self.stack.close()
        return nc


class Arena:
    def __init__(self, k, tf, tr):
        self.k, self.tf, self.tr = k, tf, tr
        self.offf = self.offr = 0

    def reset(self):
        self.offf = self.offr = 0

    def alloc(self, n, dtype=F32):
        if dtype is F32R:
            assert self.offr + n <= ARENA_R, ("arena R overflow", self.offr, n)
            ap = self.tr[:, self.offr:self.offr + n]
            self.offr += n
        else:
            assert self.offf + n <= ARENA_F, ("arena F overflow", self.offf, n)
            ap = self.tf[:, self.offf:self.offf + n]
            self.offf += n
        return ap, self.k.buf()


def r32(ap):
    return ap.bitcast(F32R)


def f32(ap):
    return ap.bitcast(F32)


def build_program(stage=99, debug=False):
    k = KB()
    nc = k.nc
    kind_s = "ExternalOutput" if debug else "Internal"

    def din(name, shape):
        return nc.dram_tensor(name, list(shape), F32, kind="ExternalInput").ap()

    def dscr(name, shape):
        return nc.dram_tensor(name, list(shape), F32, kind=kind_s).ap()

    xr = din("xr", [SC, D])
    flagc = din("flagc", [128, 1])
    ccol = din("ccol", [128, KC])
    w_ada = din("w_ada", [D, 6 * D])
    b_ada = din("b_ada", [1, 6 * D])
    w_adaf = din("w_adaf", [D, 2 * D])
    b_adaf = din("b_adaf", [1, 2 * D])
    g1col = din("g1col", [128, KC])
    g2col = din("g2col", [128, KC])
    gfrow = din("gfrow", [1, D])
    ggn = din("ggn", [128, 8])
    w_in = din("w_in", [D, 7168])
    w_out = din("w_out", [D, D])
    w_router = din("w_router", [D, NE])
    rbias = din("rbias", [128, NE])
    w_gate = din("w_gate", [NE, D, FF])
    w_up = din("w_up", [NE, D, FF])
    w_down = din("w_down", [NE, FF, D])
    ws_gate = din("ws_gate", [D, FF])
    ws_up = din("ws_up", [D, FF])
    ws_down = din("ws_down", [FF, D])
    cosT = din("cosT", [128, SC])
    sinT = din("sinT", [128, SC])
    prot = din("prot", [128, 128])
    ident_d = din("ident", [128, 128])
    ones_d = din("ones", [128, 128])
    dmatT = din("dmatT", [128, 8 * 128])
    qdec = din("qdec", [128, 8 * 128])
    kdec = din("kdec", [128, 8])
    cdec = din("cdec", [128, 8])
    pdec = din("pdec", [128, 8 * 16])
    out_d = nc.dram_tensor("out", [SO, D], F32, kind="ExternalOutput").ap()

    modb_d = dscr("modb_d", [128, 8 * D])
    hT_d = dscr("hT_d", [KC, 128, SC])
    qT_d = dscr("qT_d", [8, 128, SO])
    kT_d = dscr("kT_d", [8, 128, SC])
    v_d = dscr("v_d", [SC, 1024])
    rqT_d = dscr("rqT_d", [8, 128, SO])
    rkT_d = dscr("rkT_d", [8, 128, SC])
    rv_d = dscr("rv_d", [SC, 1024])
    rgT_d = dscr("rgT_d", [8, 128, SO])
    oT_d = dscr("oT_d", [16, 128, SO])
    x1_d = dscr("x1_d", [SO, D])
    h2T_d = dscr("h2T_d", [KC, 128, SO])
    y_d = dscr("y_d", [SO, D])

    arena_f = k.sb("arena_f", [128, ARENA_F])
    arena_r = k.sb("arena_r", [128, ARENA_R], F32R)
    ar = Arena(k, arena_f, arena_r)
    ident = k.sb("ident_sb", [128, 128])
    ones = k.sb("ones_sb", [128, 128])
    flag = k.sb("flag_sb", [128, 1])
    cols = k.sb("cols_sb", [128, 4 * KC])
    gw = k.sb("gw_sb", [128, 16 * 65])
    b_ident, b_ones, b_flag, b_cols, b_gw = (k.buf() for _ in range(5))
    PS = [k.ps("ps%d" % i, [128, 512]) for i in range(8)]
    PB = [k.buf("psb%d" % i) for i in range(8)]

    def psnext():
        i = k.pool[k.rr % len(k.pool)]
        k.rr += 1
        return PS[i], PB[i]

    def load(dst, src, dsem, wbuf, rbufs=(), q="sp", **kw):
        return k.dma(q, dst, src, dsem, reads=list(rbufs), writes=[wbuf], **kw)

    def store(dst, src, dsem, rbuf, wbufs=(), q="pool", **kw):
        return k.dma(q, dst, src, dsem, reads=[rbuf], writes=list(wbufs), **kw)

    dc = k.dsem()
    load(ident[:], ident_d, dc, b_ident)
    load(ones[:], ones_d, k.dsem(), b_ones)
    load(flag[:], flagc, k.dsem(), b_flag)

    last_store = [None]
    dram_bufs = {}

    def dbuf(name):
        if name not in dram_bufs:
            dram_bufs[name] = k.buf("dram_" + name)
        return dram_bufs[name]

    ar.reset()
    cc, b_cc = ar.alloc(KC)
    cs, b_cs = ar.alloc(KC)
    csB, b_csB = ar.alloc(KC * 128, F32R)
    CW = 512
    wt = [ar.alloc(KC * CW, F32R) for _ in range(2)]
    wds = [k.dsem() for _ in range(2)]
    bt = [ar.alloc(CW) for _ in range(2)]
    bds = [k.dsem() for _ in range(2)]
    mst = [ar.alloc(CW) for _ in range(2)]
    mds = [k.dsem() for _ in range(2)]
    load(cc, ccol, k.dsem(), b_cc)
    k.op("act", lambda e: e.activation(out=cs, in_=cc, func=AF.Silu), reads=[b_cc], writes=[b_cs])
    for kc in range(KC):
        k.op("dve", lambda e, kc=kc: e.tensor_scalar(out=csB[:, kc * 128:(kc + 1) * 128], in0=ones[:],
                                                     scalar1=cs[:, kc:kc + 1], scalar2=None, op0=ALU.mult),
             reads=[b_ones, b_cs], writes=[b_csB])
    b_modb = dbuf("modb")
    for ch in range(32):
        if ch < 24:
            wsrc, bsrc, c0 = w_ada, b_ada, ch * CW
        else:
            wsrc, bsrc, c0 = w_adaf, b_adaf, (ch - 24) * CW
        (wa, wb), wd = wt[ch % 2], wds[ch % 2]
        (ba, bb), bd = bt[ch % 2], bds[ch % 2]
        (ma, mb), md = mst[ch % 2], mds[ch % 2]
        wa3 = wa.rearrange("p (k c) -> p k c", k=KC)
        load(wa3, r32(wsrc[:, c0:c0 + CW]).rearrange("(k p) c -> p k c", p=128), wd, wb)
        load(ba[0:1, :], bsrc[:, c0:c0 + CW], bd, bb)
        pt, pb = psnext()
        for kc in range(KC):
            k.op("pe", lambda e, pt=pt, kc=kc, wa3=wa3: e.matmul(pt[:, 0:CW], lhsT=csB[:, kc * 128:(kc + 1) * 128],
                                                                 rhs=wa3[:, kc, :], start=(kc == 0), stop=False),
                 reads=[b_csB, wb], writes=[pb])
        k.op("pe", lambda e, pt=pt, ba=ba: e.matmul(pt[:, 0:CW], lhsT=ones[0:1, :], rhs=ba[0:1, :], start=False, stop=True),
             reads=[b_ones, bb], writes=[pb])
        k.op("act", lambda e, pt=pt, ma=ma: e.copy(out=ma, in_=pt[:, 0:CW]), reads=[pb], writes=[mb])
        store(modb_d[:, ch * CW:(ch + 1) * CW], ma, md, mb, wbufs=[b_modb])
    tmpc, b_tmpc = ar.alloc(6 * KC)
    gcol, b_gcol = ar.alloc(2 * KC)
    dcol = k.dsem()
    for i, blk in enumerate([0, 1, 3, 4]):
        load(tmpc[:, i * KC:(i + 1) * KC],
             modb_d[0, blk * D:(blk + 1) * D].rearrange("(k p) -> p k", p=128),
             dcol, b_tmpc, rbufs=[b_modb], allow_slow_non_contiguous=True)
    load(gcol[:, 0:KC], g1col, dcol, b_gcol)
    load(gcol[:, KC:2 * KC], g2col, dcol, b_gcol)
    for j in range(2):
        k.op("dve", lambda e, j=j: e.scalar_tensor_tensor(out=cols[:, (2 * j) * KC:(2 * j + 1) * KC],
                                                          in0=tmpc[:, (2 * j + 1) * KC:(2 * j + 2) * KC], scalar=1.0,
                                                          in1=gcol[:, j * KC:(j + 1) * KC], op0=ALU.add, op1=ALU.mult),
             reads=[b_tmpc, b_gcol], writes=[b_cols])
        k.op("dve", lambda e, j=j: e.tensor_copy(out=cols[:, (2 * j + 1) * KC:(2 * j + 2) * KC],
                                                 in_=tmpc[:, (2 * j) * KC:(2 * j + 1) * KC]),
             reads=[b_tmpc], writes=[b_cols])
    k.barrier()
    if stage <= 0:
        return finish(k, nc)

    def pipeline(items, stages):
        n = len(items)
        for s_ in range(n + len(stages) - 1):
            for d in range(len(stages) - 1, -1, -1):
                if 0 <= s_ - d < n:
                    stages[d](items[s_ - d])

    def norm_phase(ntiles, src_rows, src_rbufs, dstT, b_dst, colbase, router=None):
        xts = [ar.alloc(D) for _ in range(2)]
        xds = [k.dsem() for _ in range(2)]
        junk, b_junk = ar.alloc(D)
        xss = [ar.alloc(D) for _ in range(2)]
        stats = [ar.alloc(4) for _ in range(2)]
        hts = [ar.alloc(KC * 128, F32R) for _ in range(2)]
        hds = [k.dsem() for _ in range(2)]
        if router is not None:
            hF, b_hF = ar.alloc(KC * 128)
            hF3 = hF.rearrange("p (k t) -> p k t", k=KC)

        def n1(t):
            (xt, b_xt), (stat, b_stat) = xts[t % 2], stats[t % 2]
            load(xt, src_rows(t), xds[t % 2], b_xt, rbufs=src_rbufs)
            k.op("dve", lambda e: e.scalar_tensor_tensor(out=junk, in0=xt, scalar=1.0, in1=xt, op0=ALU.mult, op1=ALU.mult,
                                                         accum_out=stat[:, 0:1]), reads=[b_xt], writes=[b_junk, b_stat])
            k.op("dve", lambda e: e.tensor_scalar(out=stat[:, 1:2], in0=stat[:, 0:1], scalar1=1.0 / D, scalar2=EPS,
                                                  op0=ALU.mult, op1=ALU.add), reads=[b_stat], writes=[b_stat])
            k.op("act", lambda e: e.activation(out=stat[:, 2:3], in_=stat[:, 1:2], func=AF.Sqrt), reads=[b_stat], writes=[b_stat])
            k.op("dve", lambda e: e.reciprocal(out=stat[:, 3:4], in_=stat[:, 2:3]), reads=[b_stat], writes=[b_stat])

        def n2(t):
            (xt, b_xt), (stat, b_stat), (xs, b_xs) = xts[t % 2], stats[t % 2], xss[t % 2]
            k.op("act", lambda e: e.activation(out=xs, in_=xt, func=AF.Identity, scale=stat[:, 3:4]), reads=[b_xt, b_stat], writes=[b_xs])

        def n3(t):
            (xs, b_xs) = xss[t % 2]
            for g in range(4):
                pt, pb = PS[(t % 2) * 4 + g], PB[(t % 2) * 4 + g]
                for j in range(4):
                    kc = g * 4 + j
                    k.op("pe", lambda e, pt=pt, j=j, kc=kc: e.transpose(pt[:, j * 128:(j + 1) * 128], xs[:, kc * 128:(kc + 1) * 128], ident[:]),
                         reads=[b_xs, b_ident], writes=[pb])

        def n4(t):
            (ht, b_ht) = hts[t % 2]
            ht3 = ht.rearrange("p (k t) -> p k t", k=KC)
            for g in range(4):
                pt, pb = PS[(t % 2) * 4 + g], PB[(t % 2) * 4 + g]
                for j in range(4):
                    kc = g * 4 + j
                    def evac(dst3, dbuf_, eng, pt=pt, j=j, kc=kc):
                        sc_ = cols[:, colbase + kc:colbase + kc + 1]
                        bi_ = cols[:, colbase + KC + kc:colbase + KC + kc + 1]
                        if eng == "act":
                            k.op("act", lambda e: e.activation(out=dst3[:, kc, :], in_=pt[:, j * 128:(j + 1) * 128], func=AF.Identity, scale=sc_, bias=bi_),
                                 reads=[pb, b_cols], writes=[dbuf_])
                        else:
                            k.op("dve", lambda e: e.tensor_scalar(out=dst3[:, kc, :], in0=pt[:, j * 128:(j + 1) * 128], scalar1=sc_, scalar2=bi_,
                                                                  op0=ALU.mult, op1=ALU.add), reads=[pb, b_cols], writes=[dbuf_])
                    evac(ht3, b_ht, "act")
                    if router is not None:
                        evac(hF3, b_hF, "act")
            store(dstT[:, :, t * 128:(t + 1) * 128].rearrange("k p t -> p k t"), f32(ht3), hds[t % 2], b_ht, wbufs=[b_dst])

        stages = [n1, n2, n3, n4]
        if router is not None:
            stages.append(lambda t: router(t, hF3, b_hF, (PS[(t % 2) * 4], PB[(t % 2) * 4])))
        pipeline(list(range(ntiles)), stages)

    ar.reset()
    b_hTd = dbuf("hT")
    norm_phase(32, lambda t: xr[t * 128:(t + 1) * 128, :], [], hT_d, b_hTd, 0)
    k.barrier()
    if stage <= 1:
        return finish(k, nc)

    ar.reset()
    wts = [ar.alloc(KC * 256, F32R) for _ in range(2)]
    wds2 = [k.dsem() for _ in range(2)]
    hTs = [ar.alloc(KC * 512, F32R) for _ in range(3)]
    hds2 = [k.dsem() for _ in range(3)]
    cs_t, b_cost = ar.alloc(SC)
    sn_t, b_sint = ar.alloc(SC)
    prot_t, b_prot = ar.alloc(128, F32R)
    load(cs_t, cosT, k.dsem(), b_cost)
    load(sn_t, sinT, k.dsem(), b_sint)
    load(prot_t, r32(prot), k.dsem(), b_prot)
    stg = [ar.alloc(512) for _ in range(4)]
    sds = [k.dsem() for _ in range(4)]
    ut, b_ut = ar.alloc(512, F32R)
    t1, b_t1 = ar.alloc(512)
    t2, b_t2 = ar.alloc(512)
    nst = [0]
    nht = [0]
    nw = [0]
    kinds = ["q", "q", "k", "k", "v", "v", "rq", "rq", "rk", "rk", "rv", "rv", "rg", "rg"]
    for (ta_, tb__) in [(0, 1), (2, 3), (4, 5), (6, 7)]:
        own = ta_ < 4
        hcur = {}
        for tt in (ta_, tb__):
            si = nht[0] % 3
            nht[0] += 1
            (ha, hbuf) = hTs[si]
            ha3 = ha.rearrange("p (k t) -> p k t", k=KC)
            load(ha3, r32(hT_d[:, :, tt * 512:(tt + 1) * 512]).rearrange("k p t -> p k t"), hds2[si], hbuf, rbufs=[b_hTd])
            hcur[tt] = (ha3, hbuf)
        for hg in range(28):
            kind = kinds[hg // 2]
            if (not own) and kind in ("q", "rq", "rg"):
                continue
            wi = nw[0] % 2
            nw[0] += 1
            (wa, wb) = wts[wi]
            wa3 = wa.rearrange("p (k c) -> p k c", k=KC)
            load(wa3, r32(w_in[:, hg * 256:(hg + 1) * 256]).rearrange("(k p) c -> p k c", p=128), wds2[wi], wb)
            hb = (hg % 4) * 2
            for tt in (ta_, tb__):
                ha3, hbuf = hcur[tt]
                if kind in ("v", "rv"):
                    for sub in range(4):
                        pt, pb = psnext()
                        (sa, sbuf), sd = stg[nst[0] % 4], sds[nst[0] % 4]
                        nst[0] += 1
                        for kc in range(KC):
                            k.op("pe", lambda e, pt=pt, kc=kc, ha3=ha3, wa3=wa3, sub=sub: e.matmul(
                                pt[:, 0:256], lhsT=ha3[:, kc, sub * 128:(sub + 1) * 128], rhs=wa3[:, kc, :], start=(kc == 0), stop=(kc == KC - 1)),
                                reads=[hbuf, wb], writes=[pb])
                        tok0 = tt * 512 + sub * 128
                        if tok0 >= SO:
                            k.op("dve", lambda e, pt=pt, sa=sa: e.tensor_scalar(out=sa[:, 0:256], in0=pt[:, 0:256], scalar1=flag[:, 0:1], scalar2=None, op0=ALU.mult),
                                 reads=[pb, b_flag], writes=[sbuf])
                        else:
                            k.op("dve", lambda e, pt=pt, sa=sa: e.tensor_copy(out=sa[:, 0:256], in_=pt[:, 0:256]), reads=[pb], writes=[sbuf])
                        dst = v_d if kind == "v" else rv_d
                        c0 = (hg % 4) * 256
                        store(dst[tok0:tok0 + 128, c0:c0 + 256], sa[:, 0:256], sd, sbuf, wbufs=[dbuf(kind)])
                else:
                    for sub in range(2):
                        pt, pb = psnext()
                        (sa, sbuf), sd = stg[nst[0] % 4], sds[nst[0] % 4]
                        nst[0] += 1
                        for kc in range(KC):
                            k.op("pe", lambda e, pt=pt, kc=kc, ha3=ha3, wa3=wa3, sub=sub: e.matmul(
                                pt[:], lhsT=wa3[:, kc, sub * 128:(sub + 1) * 128], rhs=ha3[:, kc, :], start=(kc == 0), stop=(kc == KC - 1)),
                                reads=[hbuf, wb], writes=[pb])
                        head = hb + sub
                        tsl = slice(tt * 512, (tt + 1) * 512)
                        if kind == "q":
                            k.op("act", lambda e, pt=pt, sa=sa: e.activation(out=sa, in_=pt[:], func=AF.Copy, scale=float(128 ** -0.5)),
                                 reads=[pb], writes=[sbuf])
                            store(qT_d[head, :, tsl], sa, sd, sbuf, wbufs=[dbuf("q")])
                        elif kind == "k":
                            k.op("act", lambda e, pt=pt, sa=sa: e.copy(out=sa, in_=pt[:]), reads=[pb], writes=[sbuf])
                            store(kT_d[head, :, tsl], sa, sd, sbuf, wbufs=[dbuf("k")])
                        elif kind == "rg":
                            k.op("act", lambda e, pt=pt, sa=sa: e.activation(out=sa, in_=pt[:], func=AF.Silu), reads=[pb], writes=[sbuf])
                            store(rgT_d[head, :, tsl], sa, sd, sbuf, wbufs=[dbuf("rg")])
                        else:
                            k.op("act", lambda e, pt=pt: e.copy(out=ut, in_=pt[:]), reads=[pb], writes=[b_ut])
                            p2, pb2 = psnext()
                            k.op("pe", lambda e, p2=p2: e.matmul(p2[:], lhsT=prot_t, rhs=ut, start=True, stop=True),
                                 reads=[b_prot, b_ut], writes=[pb2])
                            k.op("dve", lambda e, tsl=tsl: e.tensor_tensor(out=t1, in0=f32(ut), in1=cs_t[:, tsl], op=ALU.mult),
                                 reads=[b_ut, b_cost], writes=[b_t1])
                            k.op("dve", lambda e, p2=p2, tsl=tsl: e.tensor_tensor(out=t2, in0=p2[:], in1=sn_t[:, tsl], op=ALU.mult),
                                 reads=[pb2, b_sint], writes=[b_t2])
                            k.op("pool", lambda e, sa=sa: e.tensor_tensor(out=sa, in0=t1, in1=t2, op=ALU.add), reads=[b_t1, b_t2], writes=[sbuf])
                            dst = rqT_d if kind == "rq" else rkT_d
                            store(dst[head, :, tsl], sa, sd, sbuf, wbufs=[dbuf(kind)])
    k.barrier()
    if stage <= 2:
        return finish(k, nc)

    ar.reset()
    qs = [ar.alloc(SO, F32R) for _ in range(2)]
    ks_ = [ar.alloc(SC, F32R) for _ in range(2)]
    vs = [ar.alloc(SC, F32R) for _ in range(2)]
    qkvd = [[k.dsem() for _ in range(3)] for _ in range(2)]
    ohs = [ar.alloc(SO) for _ in range(2)]
    ohd = [k.dsem() for _ in range(2)]
    NB = 4
    e_t = [ar.alloc(512) for _ in range(NB)]
    sp_t = [ar.alloc(512) for _ in range(NB)]
    G_t = [ar.alloc(512) for _ in range(NB)]
    t_t = [ar.alloc(512) for _ in range(NB)]
    a_t = [ar.alloc(512) for _ in range(NB)]
    aT_t = [ar.alloc(512, F32R) for _ in range(NB)]
    b_oTd = dbuf("oT")
    blocks = []
    nqt = 0
    for h in range(8):
        for qt in range(16):
            nk = 32 - qt
            nch = (nk + 3) // 4
            for c in range(nch):
                kt0 = qt + 4 * c
                ntile = min(4, 32 - kt0)
                blocks.append(dict(h=h, qt=qt, c=c, nch=nch, kt0=kt0, ntile=ntile, W=ntile * 128, i=len(blocks), nqt=nqt))
            nqt += 1
    hstate = {}

    def head_bufs(h):
        (qa, qb), (ka, kb_), (va, vb) = qs[h % 2], ks_[h % 2], vs[h % 2]
        va3 = va.rearrange("p (t e) -> p t e", t=32)
        return qa, qb, ka, kb_, va3, vb

    def stageA(B):
        h, qt, c, kt0, W, i = B["h"], B["qt"], B["c"], B["kt0"], B["W"], B["i"]
        qa, qb, ka, kb_, va3, vb = head_bufs(h)
        if qt == 0 and c == 0:
            d3 = qkvd[h % 2]
            load(qa, r32(qT_d[h]), d3[0], qb, rbufs=[dbuf("q")])
            load(ka, r32(kT_d[h]), d3[1], kb_, rbufs=[dbuf("k")])
            load(va3, r32(v_d[:, h * 128:(h + 1) * 128]).rearrange("(t p) e -> p t e", p=128), d3[2], vb, rbufs=[dbuf("v")])
        s_ = i % NB
        (ea, eb), (spa, spb) = e_t[s_], sp_t[s_]
        pz, pzb = PS[i % 3], PB[i % 3]
        k.op("pe", lambda e: e.matmul(pz[:, 0:W], lhsT=qa[:, qt * 128:(qt + 1) * 128], rhs=ka[:, kt0 * 128:kt0 * 128 + W], start=True, stop=True),
             reads=[qb, kb_], writes=[pzb])
        k.op("act", lambda e: e.activation(out=ea[:, 0:W], in_=pz[:, 0:W], func=AF.Exp), reads=[pzb], writes=[eb])
        k.op("act", lambda e: e.activation(out=spa[:, 0:W], in_=ea[:, 0:W], func=AF.Ln, bias=1.0), reads=[eb], writes=[spb])
        if c == 0:
            k.op("pool", lambda e: e.affine_select(out=spa[:, 0:128], in_=spa[:, 0:128], pattern=[[1, 128]],
                                                   compare_op=ALU.is_gt, fill=0.0, base=0, channel_multiplier=-1),
                 reads=[spb], writes=[spb])

    def stageB(B):
        c, W, i = B["c"], B["W"], B["i"]
        s_ = i % NB
        (spa, spb), (Ga, Gb), (ta, tb_) = sp_t[s_], G_t[s_], t_t[s_]
        pz, pzb = PS[i % 3], PB[i % 3]
        if c == 0:
            init, rd = 0.0, [spb]
        else:
            (Gp, Gpb) = G_t[(i - 1) % NB]
            init, rd = Gp[:, 511:512], [spb, Gpb]
        k.op("dve", lambda e: e.tensor_tensor_scan(out=Ga[:, 0:W], data0=spa[:, 0:W], data1=spa[:, 0:W], initial=init, op0=ALU.add, op1=ALU.bypass),
             reads=rd, writes=[Gb])
        k.op("dve", lambda e: e.tensor_tensor(out=ta[:, 0:W], in0=pz[:, 0:W], in1=Ga[:, 0:W], op=ALU.subtract), reads=[pzb, Gb], writes=[tb_])

    def cvars(B):
        i = B["i"]
        s_ = i % NB
        return t_t[s_], a_t[s_], aT_t[s_], (PS[3 + i % 2], PB[3 + i % 2])

    def stage3(B):
        c, W = B["c"], B["W"]
        (ta, tb_), (aa, ab), _, _ = cvars(B)
        k.op("act", lambda e: e.activation(out=aa[:, 0:W], in_=ta[:, 0:W], func=AF.Exp), reads=[tb_], writes=[ab])
        if c == 0:
            k.op("pool", lambda e: e.affine_select(out=aa[:, 0:128], in_=aa[:, 0:128], pattern=[[1, 128]],
                                                   compare_op=ALU.is_gt, fill=0.0, base=0, channel_multiplier=-1),
                 reads=[ab], writes=[ab])

    def stage4(B):
        _, (aa, ab), _, (pT, pTb) = cvars(B)
        for s in range(B["ntile"]):
            k.op("pe", lambda e, s=s: e.transpose(pT[:, s * 128:(s + 1) * 128], aa[:, s * 128:(s + 1) * 128], ident[:]),
                 reads=[ab, b_ident], writes=[pTb])

    def stage5(B):
        W, i = B["W"], B["i"]
        _, _, (aTa, aTb), (pT, pTb) = cvars(B)
        if i % 2 == 0:
            k.op("act", lambda e: e.copy(out=aTa[:, 0:W], in_=pT[:, 0:W]), reads=[pTb], writes=[aTb])
        else:
            k.op("dve", lambda e: e.tensor_copy(out=aTa[:, 0:W], in_=pT[:, 0:W]), reads=[pTb], writes=[aTb])

    def stage6(B):
        h, qt, c, nch, kt0, ntile = B["h"], B["qt"], B["c"], B["nch"], B["kt0"], B["ntile"]
        qa, qb, ka, kb_, va3, vb = head_bufs(h)
        _, _, (aTa, aTb), _ = cvars(B)
        po, pob = PS[5 + B["nqt"] % 2], PB[5 + B["nqt"] % 2]
        (oh, ohb) = ohs[h % 2]
        for s in range(ntile):
            first = (c == 0 and s == 0)
            last = (c == nch - 1) and (s == ntile - 1)
            k.op("pe", lambda e, s=s, first=first, last=last: e.matmul(po[:, 0:128], lhsT=va3[:, kt0 + s, :], rhs=aTa[:, s * 128:(s + 1) * 128],
                                                                        start=first, stop=last),
                 reads=[vb, aTb], writes=[pob])
        if c == nch - 1:
            k.op("act", lambda e: e.copy(out=oh[:, qt * 128:(qt + 1) * 128], in_=po[:, 0:128]), reads=[pob], writes=[ohb])
            if qt == 15:
                store(oT_d[h], oh, ohd[h % 2], ohb, wbufs=[b_oTd])

    nblocks = len(blocks)
    stages = [stageA, stageB, stage3, stage4, stage5, stage6]
    for s in range(nblocks + len(stages) - 1):
        for d, fn in enumerate(stages):
            if 0 <= s - d < nblocks:
                fn(blocks[s - d])
    k.pool = list(range(8))
    k.barrier()
    if stage <= 3:
        return finish(k, nc)

    ar.reset()
    rqs = [ar.alloc(SO, F32R) for _ in range(2)]
    rks = [ar.alloc(SC, F32R) for _ in range(2)]
    rvs = [ar.alloc(SC, F32R) for _ in range(2)]
    rgs = [ar.alloc(SO) for _ in range(2)]
    rd4 = [[k.dsem() for _ in range(4)] for _ in range(2)]
    oraw, b_oraw = ar.alloc(SO)
    ofin = [ar.alloc(SO) for _ in range(2)]
    ofd = [k.dsem() for _ in range(2)]
    dm_t, b_dm = ar.alloc(8 * 128)
    qd_t, b_qd = ar.alloc(8 * 128)
    kd_t, b_kd = ar.alloc(8)
    cd_t, b_cd = ar.alloc(8)
    pd_t, b_pd = ar.alloc(8 * 16)
    gg_t, b_gg = ar.alloc(8)
    onesr, b_onesr = ar.alloc(128, F32R)
    load(dm_t, dmatT, k.dsem(), b_dm)
    load(qd_t, qdec, k.dsem(), b_qd)
    load(kd_t, kdec, k.dsem(), b_kd)
    load(cd_t, cdec, k.dsem(), b_cd)
    load(pd_t, pdec, k.dsem(), b_pd)
    load(gg_t, ggn, k.dsem(), b_gg)
    load(onesr, r32(ones_d), k.dsem(), b_onesr)
    Rs = [ar.alloc(128, F32R) for _ in range(2)]
    ktk = [ar.alloc(128, F32R) for _ in range(4)]
    sm_t = [ar.alloc(128, F32R) for _ in range(4)]
    qdx = [ar.alloc(128, F32R) for _ in range(4)]
    sq_t, b_sq = ar.alloc(512, F32R)
    rs_t, b_rs = ar.alloc(512)
    on_t, b_on = ar.alloc(512)
    nk_ = [0]
    k.pool = [6]
    for h in range(8):
        (qa, qb), (ka, kb_), (va, vb), (ga, gb) = rqs[h % 2], rks[h % 2], rvs[h % 2], rgs[h % 2]
        d4 = rd4[h % 2]
        load(qa, r32(rqT_d[h]), d4[0], qb, rbufs=[dbuf("rq")])
        load(ka, r32(rkT_d[h]), d4[1], kb_, rbufs=[dbuf("rk")])
        va3 = va.rearrange("p (t e) -> p t e", t=32)
        load(va3, r32(rv_d[:, h * 128:(h + 1) * 128]).rearrange("(t p) e -> p t e", p=128), d4[2], vb, rbufs=[dbuf("rv")])
        load(ga, rgT_d[h], d4[3], gb, rbufs=[dbuf("rg")])
        pR, pRb = PS[7], PB[7]

        def p1(t, ka=ka, kb_=kb_):
            pt, pb = PS[t % 3], PB[t % 3]
            k.op("pe", lambda e: e.transpose(pt[:, 0:128], f32(ka)[:, (16 + t) * 128:(17 + t) * 128], ident[:]),
                 reads=[kb_, b_ident], writes=[pb])

        def p2(t, h=h):
            pt, pb = PS[t % 3], PB[t % 3]
            (kt, ktb) = ktk[t % 4]
            k.op("dve", lambda e: e.tensor_scalar(out=kt, in0=pt[:, 0:128], scalar1=pd_t[:, h * 16 + t:h * 16 + t + 1],
                                                  scalar2=None, op0=ALU.mult), reads=[pb, b_pd], writes=[ktb])

        def p3(t, va3=va3, vb=vb, pR=pR, pRb=pRb):
            (kt, ktb) = ktk[t % 4]
            k.op("pe", lambda e: e.matmul(pR[:, 0:128], lhsT=kt, rhs=va3[:, 16 + t, :], start=(t == 0), stop=(t == 15)),
                 reads=[ktb, vb], writes=[pRb])

        pipeline(list(range(16)), [p1, p2, p3])
        rstate = dict(ri=0)
        (R0, R0b) = Rs[0]
        k.op("dve", lambda e, R0=R0, pR=pR: e.tensor_scalar(out=R0, in0=pR[:, 0:128], scalar1=flag[:, 0:1], scalar2=None, op0=ALU.mult),
             reads=[pRb, b_flag], writes=[R0b])

        def bankA(cr):
            return PS[cr % 3], PB[cr % 3]

        def bankO(cr):
            return PS[3 + cr % 3], PB[3 + cr % 3]

        def c1(cr, ka=ka, kb_=kb_, qa=qa, qb=qb):
            csl = slice(cr * 128, (cr + 1) * 128)
            pA, pAb = bankA(cr)
            k.op("pe", lambda e: e.matmul(pA[:, 0:128], lhsT=ka[:, csl], rhs=qa[:, csl], start=True, stop=True),
                 reads=[kb_, qb], writes=[pAb])
            if cr > 0:
                k.op("pe", lambda e: e.transpose(pA[:, 128:256], f32(ka)[:, csl], ident[:]), reads=[kb_, b_ident], writes=[pAb])

        def c2(cr, h=h, qa=qa, qb=qb):
            csl = slice(cr * 128, (cr + 1) * 128)
            pA, pAb = bankA(cr)
            (sm, smb), (qx, qxb), (kt, ktb) = sm_t[cr % 4], qdx[cr % 4], ktk[cr % 4]
            k.op("dve", lambda e: e.tensor_tensor(out=sm, in0=pA[:, 0:128], in1=dm_t[:, h * 128:(h + 1) * 128], op=ALU.mult),
                 reads=[pAb, b_dm], writes=[smb])
            k.op("pool", lambda e: e.tensor_tensor(out=qx, in0=f32(qa)[:, csl], in1=qd_t[:, h * 128:(h + 1) * 128], op=ALU.mult),
                 reads=[qb, b_qd], writes=[qxb])
            if cr > 0:
                k.op("dve", lambda e: e.tensor_scalar(out=kt, in0=pA[:, 128:256], scalar1=kd_t[:, h:h + 1], scalar2=None, op0=ALU.mult),
                     reads=[pAb, b_kd], writes=[ktb])

        def c3(cr, va3=va3, vb=vb):
            pA, pAb = bankA(cr)
            pO, pOb = bankO(cr)
            (sm, smb), (kt, ktb) = sm_t[cr % 4], ktk[cr % 4]
            k.op("pe", lambda e: e.matmul(pO[:, 0:128], lhsT=va3[:, cr, :], rhs=sm, start=True, stop=False),
                 reads=[vb, smb], writes=[pOb])
            if cr > 0:
                k.op("pe", lambda e: e.matmul(pA[:, 256:384], lhsT=kt, rhs=va3[:, cr, :], start=True, stop=True),
                     reads=[ktb, vb], writes=[pAb])

        def c4(cr, h=h):
            csl = slice(cr * 128, (cr + 1) * 128)
            pA, pAb = bankA(cr)
            pO, pOb = bankO(cr)
            (qx, qxb) = qdx[cr % 4]
            (R, Rb) = Rs[rstate["ri"]]
            k.op("pe", lambda e: e.matmul(pO[:, 0:128], lhsT=R, rhs=qx, start=False, stop=True),
                 reads=[Rb, qxb], writes=[pOb])
            k.op("act", lambda e: e.copy(out=oraw[:, csl], in_=pO[:, 0:128]), reads=[pOb], writes=[b_oraw])
            if cr > 0:
                rstate["ri"] ^= 1
                (Rn, Rnb) = Rs[rstate["ri"]]
                k.op("dve", lambda e: e.scalar_tensor_tensor(out=Rn, in0=f32(R), scalar=cd_t[:, h:h + 1], in1=pA[:, 256:384],
                                                             op0=ALU.mult, op1=ALU.add),
                     reads=[Rb, pAb, b_cd], writes=[Rnb])

        pipeline(list(range(15, -1, -1)), [c1, c2, c3, c4])
        (of, ofb) = ofin[h % 2]
        for sl in range(4):
            ssl = slice(sl * 512, (sl + 1) * 512)
            k.op("dve", lambda e, ssl=ssl: e.tensor_tensor(out=sq_t, in0=oraw[:, ssl], in1=oraw[:, ssl], op=ALU.mult), reads=[b_oraw], writes=[b_sq])
            pN, pNb = psnext()
            k.op("pe", lambda e, pN=pN: e.matmul(pN[:], lhsT=onesr, rhs=sq_t, start=True, stop=True), reads=[b_onesr, b_sq], writes=[pNb])
            k.op("dve", lambda e, pN=pN: e.tensor_scalar(out=rs_t, in0=pN[:], scalar1=1.0 / 128, scalar2=EPS, op0=ALU.mult, op1=ALU.add),
                 reads=[pNb], writes=[b_rs])
            k.op("act", lambda e: e.activation(out=rs_t, in_=rs_t, func=AF.Sqrt), reads=[b_rs], writes=[b_rs])
            k.op("dve", lambda e: e.reciprocal(out=rs_t, in_=rs_t), reads=[b_rs], writes=[b_rs])
            k.op("dve", lambda e, ssl=ssl: e.tensor_tensor(out=on_t, in0=oraw[:, ssl], in1=rs_t, op=ALU.mult), reads=[b_oraw, b_rs], writes=[b_on])
            k.op("dve", lambda e, of=of, ga=ga, ssl=ssl, h=h: e.scalar_tensor_tensor(out=of[:, ssl], in0=on_t, scalar=gg_t[:, h:h + 1], in1=ga[:, ssl],
                                                                                    op0=ALU.mult, op1=ALU.mult),
                 reads=[b_on, b_gg, gb], writes=[ofb])
        store(oT_d[8 + h], of, ofd[h % 2], ofb, wbufs=[b_oTd])
    k.pool = list(range(8))
    k.barrier()
    if stage <= 4:
        return finish(k, nc)

    ar.reset()
    wos = [ar.alloc(KC * 512, F32R) for _ in range(2)]
    wod = [k.dsem() for _ in range(2)]
    g1b = [ar.alloc(512) for _ in range(2)]
    g1d = [k.dsem() for _ in range(2)]
    ots = [ar.alloc(KC * 128, F32R) for _ in range(2)]
    otd = [k.dsem() for _ in range(2)]
    xsl = [ar.alloc(512) for _ in range(2)]
    xsd = [k.dsem() for _ in range(2)]
    tm_t, b_tm = ar.alloc(512)
    x1s = [ar.alloc(512) for _ in range(2)]
    x1sd = [k.dsem() for _ in range(2)]
    b_x1d = dbuf("x1")
    n5 = 0
    for cg in range(4):
        (wa, wb), wd = wos[cg % 2], wod[cg % 2]
        wa3 = wa.rearrange("p (k c) -> p k c", k=KC)
        load(wa3, r32(w_out[:, cg * 512:(cg + 1) * 512]).rearrange("(k p) c -> p k c", p=128), wd, wb)
        (ga, gb) = g1b[cg % 2]
        load(ga, modb_d[:, 2 * D + cg * 512:2 * D + (cg + 1) * 512], g1d[cg % 2], gb, rbufs=[b_modb])
        for t in range(16):
            (oa, ob), od = ots[n5 % 2], otd[n5 % 2]
            (xa, xb), xd = xsl[n5 % 2], xsd[n5 % 2]
            (x1a, x1b), x1d_ = x1s[n5 % 2], x1sd[n5 % 2]
            n5 += 1
            oa3 = oa.rearrange("p (k t) -> p k t", k=KC)
            load(oa3, r32(oT_d[:, :, t * 128:(t + 1) * 128]).rearrange("k p t -> p k t"), od, ob, rbufs=[b_oTd])
            load(xa, xr[t * 128:(t + 1) * 128, cg * 512:(cg + 1) * 512], xd, xb)
            pt, pb = psnext()
            for kc in range(KC):
                k.op("pe", lambda e, pt=pt, oa3=oa3, wa3=wa3, kc=kc: e.matmul(pt[:], lhsT=oa3[:, kc, :], rhs=wa3[:, kc, :], start=(kc == 0), stop=(kc == KC - 1)),
                     reads=[ob, wb], writes=[pb])
            k.op("dve", lambda e, pt=pt, ga=ga: e.tensor_tensor(out=tm_t, in0=pt[:], in1=ga, op=ALU.mult), reads=[pb, gb], writes=[b_tm])
            k.op("pool", lambda e, x1a=x1a, xa=xa: e.tensor_tensor(out=x1a, in0=tm_t, in1=xa, op=ALU.add), reads=[b_tm, xb], writes=[x1b])
            store(x1_d[t * 128:(t + 1) * 128, cg * 512:(cg + 1) * 512], x1a, x1d_, x1b, wbufs=[b_x1d])
    k.barrier()
    if stage <= 5:
        return finish(k, nc)

    ar.reset()
    wr_t, b_wr = ar.alloc(KC * NE)
    rb_t, b_rb = ar.alloc(NE)
    wr3 = wr_t.rearrange("p (k c) -> p k c", k=KC)
    load(wr3, w_router.rearrange("(k p) c -> p k c", p=128), k.dsem(), b_wr)
    load(rb_t, rbias, k.dsem(), b_rb)
    s_t, b_s = ar.alloc(NE)
    ch_t, b_ch = ar.alloc(NE)
    top_t, b_top = ar.alloc(64)
    gs_t, b_gs = ar.alloc(8)
    g8_t, b_g8 = ar.alloc(8)
    gm_t, b_gm = ar.alloc(8)
    pen_t, b_pen = ar.alloc(8)
    cm_t, b_cm = ar.alloc(NE)
    e8_t, b_e8 = ar.alloc(8)
    em_t, b_em = ar.alloc(NE)
    gsel_t, b_gsel = ar.alloc(NE)
    den_t, b_den = ar.alloc(2)
    b_h2Td = dbuf("h2T")
    k.op("pool", lambda e: e.memset(gw[:], 1.0), writes=[b_gw])

    def router(t, hF3, b_hF, bank):
        pl, plb = bank
        for kc in range(KC):
            k.op("pe", lambda e, kc=kc: e.matmul(pl[:, 0:NE], lhsT=hF3[:, kc, :], rhs=wr3[:, kc, :], start=(kc == 0), stop=(kc == KC - 1)),
                 reads=[b_hF, b_wr], writes=[plb])
        k.op("act", lambda e: e.activation(out=s_t, in_=pl[:, 0:NE], func=AF.Sigmoid), reads=[plb], writes=[b_s])
        k.op("dve", lambda e: e.tensor_tensor(out=ch_t, in0=s_t, in1=rb_t, op=ALU.add), reads=[b_s, b_rb], writes=[b_ch])
        for g in range(8):
            k.op("dve", lambda e, g=g: e.max(out=top_t[:, g * 8:(g + 1) * 8], in_=ch_t[:, g * 8:(g + 1) * 8]), reads=[b_ch], writes=[b_top])
        top3 = top_t.rearrange("p (g j) -> p g j", g=8)
        k.op("dve", lambda e: e.tensor_tensor(out=gs_t.rearrange("p (g o) -> p g o", o=1), in0=top3[:, :, 0:1], in1=top3[:, :, 1:2], op=ALU.add),
             reads=[b_top], writes=[b_gs])
        k.op("dve", lambda e: e.max(out=g8_t, in_=gs_t), reads=[b_gs], writes=[b_g8])
        k.op("dve", lambda e: e.tensor_scalar(out=gm_t, in0=gs_t, scalar1=g8_t[:, 3:4], scalar2=None, op0=ALU.is_ge), reads=[b_gs, b_g8], writes=[b_gm])
        k.op("dve", lambda e: e.tensor_scalar(out=pen_t, in0=gm_t, scalar1=1e9, scalar2=-1e9, op0=ALU.mult, op1=ALU.add), reads=[b_gm], writes=[b_pen])
        cm3 = cm_t.rearrange("p (g j) -> p g j", g=8)
        ch3 = ch_t.rearrange("p (g j) -> p g j", g=8)
        k.op("dve", lambda e: e.tensor_tensor(out=cm3, in0=ch3, in1=gm_t.unsqueeze(2).broadcast_to([128, 8, 8]), op=ALU.mult),
             reads=[b_ch, b_gm], writes=[b_cm])
        k.op("dve", lambda e: e.tensor_tensor(out=cm3, in0=cm3, in1=pen_t.unsqueeze(2).broadcast_to([128, 8, 8]), op=ALU.add),
             reads=[b_cm, b_pen], writes=[b_cm])
        k.op("dve", lambda e: e.max(out=e8_t, in_=cm_t), reads=[b_cm], writes=[b_e8])
        k.op("dve", lambda e: e.tensor_scalar(out=em_t, in0=cm_t, scalar1=e8_t[:, 7:8], scalar2=None, op0=ALU.is_ge), reads=[b_cm, b_e8], writes=[b_em])
        k.op("dve", lambda e: e.tensor_tensor(out=gsel_t, in0=s_t, in1=em_t, op=ALU.mult), reads=[b_s, b_em], writes=[b_gsel])
        k.op("dve", lambda e: e.reduce_sum(out=den_t[:, 0:1], in_=gsel_t, axis=mybir.AxisListType.X), reads=[b_gsel], writes=[b_den])
        k.op("dve", lambda e: e.reciprocal(out=den_t[:, 1:2], in_=den_t[:, 0:1]), reads=[b_den], writes=[b_den])
        k.op("dve", lambda e: e.tensor_scalar(out=gw[:, t * 65:t * 65 + NE], in0=gsel_t, scalar1=den_t[:, 1:2], scalar2=2.5, op0=ALU.mult, op1=ALU.mult),
             reads=[b_gsel, b_den], writes=[b_gw])

    norm_phase(16, lambda t: x1_d[t * 128:(t + 1) * 128, :], [b_x1d], h2T_d, b_h2Td, 2 * KC, router=router)
    if debug:
        gw_dbg = nc.dram_tensor("gw_dbg", [128, 16 * 65], F32, kind="ExternalOutput").ap()
        store(gw_dbg, gw[:], k.dsem(), b_gw)
    k.barrier()
    if stage <= 6:
        return finish(k, nc)

    ar.reset()
    h2, b_h2 = ar.alloc(KC * 512, F32R)
    h2d = k.dsem()
    yacc, b_yacc = ar.alloc(4 * D)
    yd = k.dsem()
    acts = [ar.alloc(4 * 512, F32R) for _ in range(2)]
    NSLOT = 6
    slots = [ar.alloc(4096, F32R) for _ in range(NSLOT)]
    sld = [k.dsem() for _ in range(NSLOT)]
    nsl = [0]
    GB = [(PS[i], PB[i]) for i in range(4)]
    UB = [(PS[i], PB[i]) for i in range(4, 8)]
    b_yd = dbuf("y")
    h23 = h2.rearrange("p (k t) -> p k t", k=KC)
    yacc3 = yacc.rearrange("p (t d) -> p t d", t=4)

    def wsrc(e, which):
        if e < NE:
            return {"g": w_gate[e], "u": w_up[e], "d": w_down[e]}[which]
        return {"g": ws_gate, "u": ws_up, "d": ws_down}[which]

    def load_gu(e, which):
        res = []
        src = r32(wsrc(e, which)).rearrange("(k p) c -> p k c", p=128)
        base = 0 if which == "g" else 2
        for hf in range(2):
            i = base + hf
            (sa, sbuf) = slots[i]
            sa3 = sa.rearrange("p (k c) -> p k c", k=8)
            load(sa3, src[:, hf * 8:(hf + 1) * 8, :], sld[i], sbuf)
            res.append((sa3, sbuf))
        return res

    def load_d(e):
        res = []
        src = r32(wsrc(e, "d")).rearrange("(k p) c -> p k c", p=128)
        for hf in range(2):
            i = 4 + hf
            (sa, sbuf) = slots[i]
            sa3 = sa.rearrange("p (k c) -> p k c", k=4)
            load(sa3, src[:, :, hf * 1024:(hf + 1) * 1024], sld[i], sbuf)
            res.append((sa3, sbuf))
        return res

    def gu_matmuls(halves, banks):
        for hf in range(2):
            sa3, sbuf = halves[hf]
            for k8 in range(8):
                kc = hf * 8 + k8
                for fc in range(4):
                    pt, pb = banks[fc]
                    k.op("pe", lambda e, pt=pt, sa3=sa3, k8=k8, fc=fc, kc=kc: e.matmul(
                        pt[:], lhsT=sa3[:, k8, fc * 128:(fc + 1) * 128], rhs=h23[:, kc, :], start=(kc == 0), stop=(kc == KC - 1)),
                        reads=[sbuf, b_h2], writes=[pb])

    def down(e, p, dh, act3, actb, first):
        nd = 0
        for hf in range(2):
            sa3, sbuf = dh[hf]
            for tb in range(4):
                for dc2 in range(2):
                    pt, pb = GB[nd % 4]
                    nd += 1
                    for fc in range(4):
                        k.op("pe", lambda e_, pt=pt, act3=act3, sa3=sa3, fc=fc, tb=tb, dc2=dc2: e_.matmul(
                            pt[:], lhsT=act3[:, fc, tb * 128:(tb + 1) * 128], rhs=sa3[:, fc, dc2 * 512:(dc2 + 1) * 512],
                            start=(fc == 0), stop=(fc == 3)), reads=[actb, sbuf], writes=[pb])
                    dsl = slice(hf * 1024 + dc2 * 512, hf * 1024 + (dc2 + 1) * 512)
                    tile = p * 4 + tb
                    gcol_ = gw[:, tile * 65 + e:tile * 65 + e + 1]
                    if first:
                        k.op("dve", lambda e_, pt=pt, tb=tb, dsl=dsl, gcol_=gcol_: e_.tensor_scalar(
                            out=yacc3[:, tb, dsl], in0=pt[:], scalar1=gcol_, scalar2=None, op0=ALU.mult),
                            reads=[pb, b_gw], writes=[b_yacc])
                    else:
                        k.op("dve", lambda e_, pt=pt, tb=tb, dsl=dsl, gcol_=gcol_: e_.scalar_tensor_tensor(
                            out=yacc3[:, tb, dsl], in0=pt[:], scalar=gcol_, in1=yacc3[:, tb, dsl], op0=ALU.mult, op1=ALU.add),
                            reads=[pb, b_gw, b_yacc], writes=[b_yacc])

    NEXP = NE + 1
    for p in range(4):
        load(h23, r32(h2T_d[:, :, p * 512:(p + 1) * 512]).rearrange("k p t -> p k t"), h2d, b_h2, rbufs=[b_h2Td])
        pend = None
        gh = load_gu(0, "g")
        uh = load_gu(0, "u")
        for e in range(NEXP):
            gu_matmuls(gh, GB)
            (aa, ab) = acts[e % 2]
            for fc in range(4):
                pt, pb = GB[fc]
                k.op("act", lambda e_, pt=pt, fc=fc, aa=aa: e_.activation(out=aa[:, fc * 512:(fc + 1) * 512], in_=pt[:], func=AF.Silu),
                     reads=[pb], writes=[ab])
            gu_matmuls(uh, UB)
            for fc in range(4):
                pt, pb = UB[fc]
                k.op("dve", lambda e_, pt=pt, fc=fc, aa=aa: e_.tensor_tensor(out=aa[:, fc * 512:(fc + 1) * 512], in0=pt[:], in1=f32(aa)[:, fc * 512:(fc + 1) * 512], op=ALU.mult),
                     reads=[pb, ab], writes=[ab])
            if e + 1 < NEXP:
                gh = load_gu(e + 1, "g")
                uh = load_gu(e + 1, "u")
            if pend is not None:
                down(*pend)
            dh = load_d(e)
            pend = (e, p, dh, aa.rearrange("p (f t) -> p f t", f=4), ab, e == 0)
        down(*pend)
        for tb in range(4):
            tile = p * 4 + tb
            store(y_d[tile * 128:(tile + 1) * 128, :], yacc3[:, tb, :], yd, b_yacc, wbufs=[b_yd])
    k.barrier()
    if stage <= 7:
        return finish(k, nc)

    ar.reset()
    g2b, b_g2b = ar.alloc(D)
    afb, b_afb = ar.alloc(D)
    shfb, b_shfb = ar.alloc(D)
    yts = [ar.alloc(D) for _ in range(2)]
    ytd = [k.dsem() for _ in range(2)]
    x1t = [ar.alloc(D) for _ in range(2)]
    x1td = [k.dsem() for _ in range(2)]
    otd_ = [k.dsem() for _ in range(2)]
    stat, b_stat = ar.alloc(4)
    load(g2b, modb_d[:, 5 * D:6 * D], k.dsem(), b_g2b, rbufs=[b_modb])
    load(shfb, modb_d[:, 6 * D:7 * D], k.dsem(), b_shfb, rbufs=[b_modb])
    load(afb, modb_d[:, 7 * D:8 * D], k.dsem(), b_afb, rbufs=[b_modb])
    gfb, b_gfb = yts[1]
    load(gfb, gfrow.partition_broadcast(128), ytd[1], b_gfb)
    k.op("dve", lambda e: e.scalar_tensor_tensor(out=afb, in0=afb, scalar=1.0, in1=gfb, op0=ALU.add, op1=ALU.mult),
         reads=[b_afb, b_gfb], writes=[b_afb])
    def f1(t):
        (ya, yb), (xa, xb) = yts[t % 2], x1t[t % 2]
        load(ya, y_d[t * 128:(t + 1) * 128, :], ytd[t % 2], yb, rbufs=[b_yd])
        load(xa, x1_d[t * 128:(t + 1) * 128, :], x1td[t % 2], xb, rbufs=[b_x1d])
        k.op("pool", lambda e: e.tensor_tensor(out=ya, in0=ya, in1=g2b, op=ALU.mult), reads=[yb, b_g2b], writes=[yb])

    def f2(t):
        (ya, yb), (xa, xb) = yts[t % 2], x1t[t % 2]
        k.op("dve", lambda e: e.tensor_tensor(out=ya, in0=ya, in1=xa, op=ALU.add), reads=[yb, xb], writes=[yb])
        k.op("dve", lambda e: e.scalar_tensor_tensor(out=xa, in0=ya, scalar=1.0, in1=ya, op0=ALU.mult, op1=ALU.mult, accum_out=stat[:, 0:1]),
             reads=[yb], writes=[xb, b_stat])
        k.op("dve", lambda e: e.tensor_scalar(out=stat[:, 1:2], in0=stat[:, 0:1], scalar1=1.0 / D, scalar2=EPS, op0=ALU.mult, op1=ALU.add),
             reads=[b_stat], writes=[b_stat])
        k.op("act", lambda e: e.activation(out=stat[:, 2:3], in_=stat[:, 1:2], func=AF.Sqrt), reads=[b_stat], writes=[b_stat])
        k.op("dve", lambda e: e.reciprocal(out=stat[:, 3:4], in_=stat[:, 2:3]), reads=[b_stat], writes=[b_stat])

    def f3(t):
        (ya, yb), (xa, xb) = yts[t % 2], x1t[t % 2]
        k.op("dve", lambda e: e.scalar_tensor_tensor(out=xa, in0=ya, scalar=stat[:, 3:4], in1=afb, op0=ALU.mult, op1=ALU.mult),
             reads=[yb, b_stat, b_afb], writes=[xb])
        k.op("pool", lambda e: e.tensor_tensor(out=xa, in0=xa, in1=shfb, op=ALU.add), reads=[xb, b_shfb], writes=[xb])
        last_store[0] = store(out_d[t * 128:(t + 1) * 128, :], xa, otd_[t % 2], xb)

    pipeline(list(range(16)), [f1, f2, f3])
    k.barrier()
    return finish(k, nc)


def finish(k, nc):
    k.barrier()
    k.build()
    return nc


def _const_tables(hf):
    H, C = 8, 128
    scale = 128.0 ** -0.5
    r = np.arange(SC)
    if hf == 1:
        pos = (SC - 1 - r).astype(np.float64)
    else:
        pos = np.where(r < SO, SO - 1 - r, 0).astype(np.float64)
    inv_freq = (10000.0 ** (-np.arange(0, 128, 2, dtype=np.float32) / 128)).astype(np.float32)
    ang = (pos.astype(np.float32)[None, :] * np.tile(inv_freq, 2)[:, None]).astype(np.float32)
    cosT = np.cos(ang).astype(np.float32)
    sinT = np.sin(ang).astype(np.float32)
    prot = np.zeros((128, 128), np.float32)
    for m in range(64):
        prot[m + 64, m] = -1.0
        prot[m, m + 64] = 1.0
    log_g = np.log(1.0 - np.exp2(-5.0 - np.arange(H, dtype=np.float64)))
    ip = np.arange(C, dtype=np.float64)
    dmatT = np.zeros((128, H, 128), np.float64)
    qdec = np.zeros((128, H, 128), np.float64)
    for h in range(H):
        diff = ip[:, None] - ip[None, :]
        dmatT[:, h, :] = np.where(diff >= 0, np.exp(np.where(diff >= 0, diff, 0) * log_g[h]), 0.0) * scale
        qdec[:, h, :] = np.exp((128.0 - ip)[None, :] * log_g[h])
    kdec = np.exp(ip[:, None] * log_g[None, :]) * scale
    cdec = np.tile(np.exp(C * log_g)[None, :], (128, 1))
    pd = np.zeros((128, H, 16), np.float64)
    for t in range(16):
        pd[:, :, t] = np.exp((t * 128 + ip)[:, None] * log_g[None, :]) * scale
    return dict(cosT=cosT, sinT=sinT, prot=prot,
                dmatT=dmatT.reshape(128, -1).astype(np.float32), qdec=qdec.reshape(128, -1).astype(np.float32),
                kdec=kdec.astype(np.float32), cdec=cdec.astype(np.float32), pdec=pd.reshape(128, -1).astype(np.float32))


def make_in_maps(x, c, w_ada, b_ada, norm1_g, w_in, ret_gn_g, w_out, norm2_g, w_router, router_bias,
                 w_gate, w_up, w_down, ws_gate, ws_up, ws_down, w_ada_final, b_ada_final, norm_f_g):
    f = lambda a: np.ascontiguousarray(np.asarray(a, dtype=np.float32))
    col = lambda v: f(np.asarray(v).reshape(-1, 128).T)
    shared = dict(
        w_ada=f(w_ada[0]), b_ada=f(b_ada[0]).reshape(1, -1), w_adaf=f(w_ada_final), b_adaf=f(b_ada_final).reshape(1, -1),
        g1col=col(norm1_g[0]), g2col=col(norm2_g[0]), gfrow=f(norm_f_g).reshape(1, -1), ggn=col(ret_gn_g[0]),
        w_in=f(w_in[0]), w_out=f(w_out[0]), w_router=f(w_router[0]),
        rbias=f(np.broadcast_to(np.asarray(router_bias[0]).reshape(1, -1), (128, NE))),
        w_gate=f(w_gate[0]), w_up=f(w_up[0]), w_down=f(w_down[0]),
        ws_gate=f(ws_gate[0]), ws_up=f(ws_up[0]), ws_down=f(ws_down[0]),
        ident=np.eye(128, dtype=np.float32), ones=np.ones((128, 128), np.float32),
    )
    tabs = [_const_tables(0), _const_tables(1)]
    x = np.asarray(x)
    in_maps = []
    for core in range(8):
        b, hf = core // 2, core % 2
        if hf == 1:
            xrv = x[b, ::-1]
        else:
            own = x[b, SO - 1::-1]
            xrv = np.concatenate([own, own], axis=0)
        m = dict(shared)
        m.update(tabs[hf])
        m["xr"] = f(xrv)
        m["flagc"] = np.full((128, 1), float(hf), np.float32)
        m["ccol"] = col(np.asarray(c)[b])
        in_maps.append(m)
    return in_maps


def kernel(**inputs):
    in_maps = make_in_maps(**inputs)
    nc = build_program()
    res = run_bass_kernel_spmd(nc, in_maps, core_ids=list(range(8)))
    out = np.zeros((4, 4096, D), np.float32)
    for core in range(8):
        b, hf = core // 2, core % 2
        o = np.asarray(res.results[core]["out"])
        out[b, hf * SO:(hf + 1) * SO] = o[::-1]
    return out
```
